# Optimizing a Trainium2 kernel written in Bass

```python
import jax
import jax.numpy as jnp
from jax import lax
import numpy as np

D_MODEL = 1024
BATCH = 8
SEQ = 2048
DEPTH = 1

MLA_HEADS = 8
MLA_Q_RANK = 256
MLA_KV_RANK = 128
MLA_NOPE = 64
MLA_ROPE = 32
MLA_V = 64
ROPE_THETA = 10000.0
DSA_HEADS = 8
DSA_KV_HEADS = 2
DSA_HEAD_DIM = 64
IDX_HEADS = 8
IDX_DIM = 32
IDX_TOPK_MAX = 256
N_EXPERTS = 32
TOP_K = 4
D_EXPERT = D_MODEL
SWIGLU_ALPHA = 1.702
SWIGLU_LIMIT = 7.0

NORM_EPS = 1e-6
NEG_INF = -1e30
Q_BLOCK = 128
MOE_BLOCK = 128

MIX_WIDTH = MLA_HEADS * MLA_V + DSA_HEADS * DSA_HEAD_DIM
IN_SPLITS = (MLA_Q_RANK, MLA_KV_RANK, MLA_ROPE,
             DSA_HEADS * DSA_HEAD_DIM, DSA_KV_HEADS * DSA_HEAD_DIM, DSA_KV_HEADS * DSA_HEAD_DIM,
             IDX_HEADS * IDX_DIM, IDX_DIM, IDX_HEADS)
IN_WIDTH = sum(IN_SPLITS)

kernel_name = "hybrid_mla_dsa_moe_block"


def rms_norm(x, g):
    xf = x.astype(jnp.float32)
    y = xf * lax.rsqrt(jnp.mean(xf * xf, axis=-1, keepdims=True) + NORM_EPS)
    return (y * g.astype(jnp.float32)).astype(x.dtype)


def rope(x, pos):
    half = x.shape[-1] // 2
    freqs = ROPE_THETA ** (-jnp.arange(half, dtype=jnp.float32) / half)
    ang = pos.astype(jnp.float32)[..., None] * freqs
    ang = ang.reshape(ang.shape[:2] + (1,) * (x.ndim - 3) + (half,))
    cos, sin = jnp.cos(ang), jnp.sin(ang)
    x1 = x[..., :half].astype(jnp.float32)
    x2 = x[..., half:].astype(jnp.float32)
    return jnp.concatenate([x1 * cos - x2 * sin, x2 * cos + x1 * sin], axis=-1).astype(x.dtype)


def to_blocks(a):
    b, s = a.shape[:2]
    return jnp.moveaxis(a.reshape((b, s // Q_BLOCK, Q_BLOCK) + a.shape[2:]), 1, 0)


def from_blocks(a):
    a = jnp.moveaxis(a, 0, 1)
    return a.reshape((a.shape[0], a.shape[1] * a.shape[2]) + a.shape[3:])


def mla_attention(c_q, c_kv, k_rope_in, pos, g_q_a, g_kv_a, w_q_b, w_kv_b):
    b, s, _ = c_q.shape
    q = (rms_norm(c_q, g_q_a) @ w_q_b).reshape(b, s, MLA_HEADS, MLA_NOPE + MLA_ROPE)
    q_nope = q[..., :MLA_NOPE]
    q_rope = rope(q[..., MLA_NOPE:], pos)
    kv = (rms_norm(c_kv, g_kv_a) @ w_kv_b).reshape(b, s, MLA_HEADS, MLA_NOPE + MLA_V)
    k_nope, v = kv[..., :MLA_NOPE], kv[..., MLA_NOPE:]
    k_rope = rope(k_rope_in, pos)
    scale = (MLA_NOPE + MLA_ROPE) ** -0.5
    key_idx = jnp.arange(s)

    def block(args):
        qn, qr, qi = args
        sc = (jnp.einsum('bqhd,bkhd->bhqk', qn, k_nope)
              + jnp.einsum('bqhd,bkd->bhqk', qr, k_rope)).astype(jnp.float32) * scale
        sc = jnp.where(key_idx[None, None, None, :] <= qi[None, None, :, None], sc, NEG_INF)
        p = jax.nn.softmax(sc, axis=-1).astype(v.dtype)
        return jnp.einsum('bhqk,bkhd->bqhd', p, v)

    q_ids = jnp.arange(s).reshape(-1, Q_BLOCK)
    out = lax.map(block, (to_blocks(q_nope), to_blocks(q_rope), q_ids))
    return from_blocks(out).reshape(b, s, MLA_HEADS * MLA_V)


def dsa_attention(q, k, v, q_i, k_i, w_i, pos):
    b, s, _ = q.shape
    grp = DSA_HEADS // DSA_KV_HEADS
    q = q.reshape(b, s, DSA_KV_HEADS, grp, DSA_HEAD_DIM)
    k = k.reshape(b, s, DSA_KV_HEADS, DSA_HEAD_DIM)
    v = v.reshape(b, s, DSA_KV_HEADS, DSA_HEAD_DIM)
    q_i = q_i.reshape(b, s, IDX_HEADS, IDX_DIM) * (IDX_DIM ** -0.5)
    w_i = w_i * (IDX_HEADS ** -0.5)
    n_sel = min(IDX_TOPK_MAX, s // 4)
    key_idx = jnp.arange(s)
    slopes = jnp.exp2(-8.0 * jnp.arange(1, DSA_HEADS + 1, dtype=jnp.float32) / DSA_HEADS)
    slopes = slopes.reshape(DSA_KV_HEADS, grp)
    gather = jax.vmap(lambda a, i: a[i])

    def block(args):
        qb, qib, wib, posb, ti = args
        rel = jax.nn.relu(jnp.einsum('bqhd,bsd->bqsh', qib, k_i).astype(jnp.float32))
        idx_score = jnp.einsum('bqsh,bqh->bqs', rel, wib.astype(jnp.float32))
        idx_score = jnp.where(key_idx[None, None, :] <= ti[None, :, None], idx_score, NEG_INF)
        _, sel = lax.top_k(idx_score, n_sel)
        k_sel = gather(k, sel)
        v_sel = gather(v, sel)
        pos_sel = gather(pos, sel)
        sc = jnp.einsum('bqhgd,bqnhd->bqhgn', qb, k_sel).astype(jnp.float32) * (DSA_HEAD_DIM ** -0.5)
        dist = jnp.abs(posb[:, :, None] - pos_sel).astype(jnp.float32)
        sc = sc - slopes[None, None, :, :, None] * dist[:, :, None, None, :]
        valid = sel <= ti[None, :, None]
        sc = jnp.where(valid[:, :, None, None, :], sc, NEG_INF)
        p = jax.nn.softmax(sc, axis=-1).astype(v.dtype)
        return jnp.einsum('bqhgn,bqnhd->bqhgd', p, v_sel)

    q_ids = jnp.arange(s).reshape(-1, Q_BLOCK)
    out = lax.map(block, (to_blocks(q), to_blocks(q_i), to_blocks(w_i), to_blocks(pos), q_ids))
    return from_blocks(out).reshape(b, s, DSA_HEADS * DSA_HEAD_DIM)


def moe_ffn(h, w_router, b_router, w_mlp1, b_mlp1, w_mlp2, b_mlp2):
    b, s, d = h.shape
    xf = h.reshape(-1, d)
    n = xf.shape[0]
    logits = (xf @ w_router + b_router).astype(jnp.float32)
    top_val, top_idx = lax.top_k(logits, TOP_K)
    gates = jax.nn.softmax(top_val, axis=-1)
    n_assign = n * TOP_K
    flat_e = top_idx.reshape(-1)
    flat_tok = jnp.arange(n_assign, dtype=jnp.int32) // TOP_K
    flat_gate = gates.reshape(-1)
    order = jnp.argsort(flat_e)
    e_sorted = flat_e[order]
    counts = jnp.bincount(flat_e, length=N_EXPERTS)
    start = jnp.cumsum(counts) - counts
    padded = (counts + MOE_BLOCK - 1) // MOE_BLOCK * MOE_BLOCK
    pad_end = jnp.cumsum(padded)
    pad_start = pad_end - padded
    dest = pad_start[e_sorted] + jnp.arange(n_assign) - start[e_sorted]
    n_slots = (n_assign + MOE_BLOCK - 1) // MOE_BLOCK * MOE_BLOCK + N_EXPERTS * MOE_BLOCK
    n_blocks = n_slots // MOE_BLOCK
    slot_tok = jnp.full((n_slots,), n, jnp.int32).at[dest].set(flat_tok[order])
    slot_gate = jnp.zeros((n_slots,), jnp.float32).at[dest].set(flat_gate[order])
    block_e = jnp.minimum(
        jnp.searchsorted(pad_end, jnp.arange(n_blocks) * MOE_BLOCK, side='right'), N_EXPERTS - 1)
    x_pad = jnp.concatenate([xf, jnp.zeros((1, d), xf.dtype)], axis=0)
    x_slots = x_pad[slot_tok].reshape(n_blocks, MOE_BLOCK, d)

    def expert_block(args):
        xb, e = args
        hid = xb @ w_mlp1[e] + b_mlp1[e]
        glu = jnp.minimum(hid[..., ::2], SWIGLU_LIMIT)
        lin = jnp.clip(hid[..., 1::2], -SWIGLU_LIMIT, SWIGLU_LIMIT)
        act = glu * jax.nn.sigmoid(SWIGLU_ALPHA * glu) * (lin + 1.0)
        return act @ w_mlp2[e] + b_mlp2[e]

    y = lax.map(expert_block, (x_slots, block_e)).reshape(n_slots, d)
    y = y * slot_gate[:, None].astype(y.dtype)
    out = jax.ops.segment_sum(y, slot_tok, num_segments=n + 1)[:n]
    return out.reshape(b, s, d)


def setup_inputs(seed: int = 0) -> dict:
    key = jax.random.key(seed)
    ks = jax.random.split(key, 24)
    f32 = jnp.float32
    L, D, F, E = DEPTH, D_MODEL, D_EXPERT, N_EXPERTS

    def nrm(k, shape, scale):
        return jax.random.normal(k, shape, f32) * scale

    def gain(k, width):
        return 1.0 + nrm(k, (L, width), 0.02)

    x = nrm(ks[0], (BATCH, SEQ, D), 1.0)
    c = nrm(ks[1], (BATCH, D), 1.0)
    offsets = jax.random.randint(ks[2], (BATCH, 1), 0, 4096, dtype=jnp.int32)
    positions = offsets + jnp.arange(SEQ, dtype=jnp.int32)[None, :]
    return {
        "x": x,
        "c": c,
        "positions": positions,
        "w_ada": nrm(ks[3], (L, D, 6 * D), 0.2 * D ** -0.5),
        "b_ada": nrm(ks[4], (L, 6 * D), 0.01),
        "g_pre_mix": gain(ks[5], D),
        "g_post_mix": gain(ks[6], D),
        "g_pre_ffn": gain(ks[7], D),
        "g_post_ffn": gain(ks[8], D),
        "w_in": nrm(ks[9], (L, D, IN_WIDTH), D ** -0.5),
        "g_q_a": gain(ks[10], MLA_Q_RANK),
        "g_kv_a": gain(ks[11], MLA_KV_RANK),
        "w_q_b": nrm(ks[12], (L, MLA_Q_RANK, MLA_HEADS * (MLA_NOPE + MLA_ROPE)), MLA_Q_RANK ** -0.5),
        "w_kv_b": nrm(ks[13], (L, MLA_KV_RANK, MLA_HEADS * (MLA_NOPE + MLA_V)), MLA_KV_RANK ** -0.5),
        "w_o": nrm(ks[14], (L, MIX_WIDTH, D), MIX_WIDTH ** -0.5),
        "w_router": nrm(ks[15], (L, D, E), D ** -0.5),
        "b_router": nrm(ks[16], (L, E), 0.01),
        "w_mlp1": nrm(ks[17], (L, E, D, 2 * F), D ** -0.5),
        "b_mlp1": nrm(ks[18], (L, E, 2 * F), 0.01),
        "w_mlp2": nrm(ks[19], (L, E, F, D), F ** -0.5),
        "b_mlp2": nrm(ks[20], (L, E, D), 0.01),
    }


def reference(x, c, positions, w_ada, b_ada, g_pre_mix, g_post_mix, g_pre_ffn, g_post_ffn,
              w_in, g_q_a, g_kv_a, w_q_b, w_kv_b, w_o, w_router, b_router,
              w_mlp1, b_mlp1, w_mlp2, b_mlp2):
    split_at = np.cumsum(IN_SPLITS)[:-1].tolist()
    cond = jax.nn.silu(c)
    for l in range(DEPTH):
        mod = cond @ w_ada[l] + b_ada[l]
        sh1, sc1, gt1, sh2, sc2, gt2 = [m[:, None, :] for m in jnp.split(mod, 6, axis=-1)]

        h = rms_norm(x, g_pre_mix[l]) * (1.0 + sc1) + sh1
        proj = h @ w_in[l]
        (c_q, c_kv, k_rope, q_d, k_d, v_d, q_idx, k_idx, w_idx) = jnp.split(proj, split_at, axis=-1)
        y_mla = mla_attention(c_q, c_kv, k_rope, positions, g_q_a[l], g_kv_a[l], w_q_b[l], w_kv_b[l])
        y_dsa = dsa_attention(q_d, k_d, v_d, q_idx, k_idx, w_idx, positions)
        mix = jnp.concatenate([y_mla, y_dsa], axis=-1) @ w_o[l]
        x = x + gt1 * rms_norm(mix, g_post_mix[l])

        h2 = rms_norm(x, g_pre_ffn[l]) * (1.0 + sc2) + sh2
        ffn = moe_ffn(h2, w_router[l], b_router[l], w_mlp1[l], b_mlp1[l], w_mlp2[l], b_mlp2[l])
        x = x + gt2 * rms_norm(ffn, g_post_ffn[l])
    return x
```

```python
import os
import math
from contextlib import ExitStack
import numpy as np
import concourse.bass as bass
import concourse.mybir as mybir
from concourse.bass_utils import run_bass_kernel_spmd

F32 = mybir.dt.float32
BF16 = mybir.dt.bfloat16
I32 = mybir.dt.int32
ALU = mybir.AluOpType
AF = mybir.ActivationFunctionType
AX = mybir.AxisListType

S_LEN = 2048
D = 1024
NT = 16
NC4 = 4
EPS = 1e-6
NEXP = 32


class Sched:
    ENG = ('pe', 'act', 'dve', 'pool', 'sp')

    def __init__(self, nc, es):
        self.nc = nc
        self.es = es
        self.sem = {}
        self.count = {}
        self.seen = {}
        self.stream = {e: [] for e in self.ENG}
        for e in self.ENG:
            self.sem[e] = es.enter_context(nc.semaphore('c_' + e))
            self.count[e] = 0
            self.seen[e] = {}
        self.last_w = {}
        self.readers = {}
        self.nchan = 0

    def chan(self, name=None):
        self.nchan += 1
        key = 'ch%d_%s' % (self.nchan, name or '')
        self.sem[key] = self.es.enter_context(self.nc.semaphore('d%d' % self.nchan))
        self.count[key] = 0
        return key

    def _deps(self, eng, reads, writes):
        deps = {}

        def add(e, i):
            if i > deps.get(e, 0):
                deps[e] = i
        for k in reads:
            lw = self.last_w.get(k)
            if lw is not None:
                add(*lw)
        for k in writes:
            lw = self.last_w.get(k)
            if lw is not None:
                add(*lw)
            for e, i in self.readers.get(k, {}).items():
                if e != eng:
                    add(e, i)
        return deps

    def _emit_waits(self, eng, deps):
        seen = self.seen[eng]
        for e, i in deps.items():
            if seen.get(e, 0) >= i:
                continue
            seen[e] = i
            self.stream[eng].append(('wait', self.sem[e], i))

    def _mark(self, who, idx, reads, writes):
        for k in reads:
            self.readers.setdefault(k, {})[who] = idx
        for k in writes:
            self.last_w[k] = (who, idx)
            self.readers[k] = {}

    def op(self, eng, fn, reads=(), writes=()):
        deps = self._deps(eng, reads, writes)
        self._emit_waits(eng, deps)
        self.count[eng] += 1
        self.stream[eng].append(('inst', fn, self.sem[eng], 1))
        self._mark(eng, self.count[eng], reads, writes)

    def group(self, eng, fns, reads=(), writes=()):
        deps = self._deps(eng, reads, writes)
        self._emit_waits(eng, deps)
        self.count[eng] += 1
        for f in fns[:-1]:
            self.stream[eng].append(('inst', f, None, 0))
        self.stream[eng].append(('inst', fns[-1], self.sem[eng], 1))
        self._mark(eng, self.count[eng], reads, writes)

    def dma(self, eng, ch, fn, reads=(), writes=(), drain=False):
        deps = self._deps(ch, reads, writes)
        if drain and self.count[ch] > 0:
            deps[ch] = self.count[ch]
        self._emit_waits(eng, deps)
        self.count[ch] += 16
        self.stream[eng].append(('inst', fn, self.sem[ch], 16))
        self._mark(ch, self.count[ch], reads, writes)

    def barrier(self):
        for eng in self.ENG:
            deps = {e: c for e, c in self.count.items() if c > 0 and e not in getattr(self, 'hold', ())}
            self._emit_waits(eng, deps)
        self.last_w = {}
        self.readers = {}

    def finish(self):
        for eng in self.ENG:
            deps = {e: c for e, c in self.count.items() if c > 0}
            self._emit_waits(eng, deps)

    def replay(self):
        nc = self.nc
        streams = self.stream

        def run(engobj, lst):
            for it in lst:
                if it[0] == 'wait':
                    engobj.wait_ge(it[1], it[2])
                else:
                    ins = it[1](engobj)
                    if it[2] is not None:
                        ins.then_inc(it[2], it[3])
        with nc.Block() as block:
            @block.tensor
            def _(e):
                run(e, streams['pe'])

            @block.scalar
            def _(e):
                run(e, streams['act'])

            @block.vector
            def _(e):
                run(e, streams['dve'])

            @block.gpsimd
            def _(e):
                run(e, streams['pool'])

            @block.sync
            def _(e):
                run(e, streams['sp'])


def MM(out, lhsT, rhs, start=True, stop=True):
    return lambda e: e.matmul(out, lhsT=lhsT, rhs=rhs, start=start, stop=stop)


def TR(out, in_, ident):
    return lambda e: e.transpose(out=out, in_=in_, identity=ident)


def ACTV(out, in_, func, **kw):
    return lambda e: e.activation(out=out, in_=in_, func=func, **kw)


def TS(out, in0, s1, s2, op0, op1=None, **kw):
    if op1 is None:
        return lambda e: e.tensor_scalar(out=out, in0=in0, scalar1=s1, scalar2=None, op0=op0, **kw)
    return lambda e: e.tensor_scalar(out=out, in0=in0, scalar1=s1, scalar2=s2, op0=op0, op1=op1, **kw)


def TT(out, in0, in1, op):
    return lambda e: e.tensor_tensor(out=out, in0=in0, in1=in1, op=op)


def STT(out, in0, scalar, in1, op0, op1, **kw):
    return lambda e: e.scalar_tensor_tensor(out=out, in0=in0, scalar=scalar, in1=in1, op0=op0, op1=op1, **kw)


def CP(out, in_):
    return lambda e: e.tensor_copy(out=out, in_=in_)


def MS(ap, v):
    return lambda e: e.memset(ap, v)


def DMA(out, in_):
    return lambda e: e.dma_start(out=out, in_=in_)


def build_program(stage=99, dbg=False):
    nc = bass.Bass("TRN2", target_bir_lowering=False)

    def din(name, shape, dt):
        return nc.dram_tensor(name, shape, dt, kind="ExternalInput").ap()
    x_d = din("x", [S_LEN, D], F32)
    c_d = din("c", [128, 8], F32)
    posr_d = din("posr", [S_LEN], I32)
    posc_d = din("posc", [128, NT], I32)
    frq_d = din("frq", [128, 1], F32)
    w_ada_d = din("w_ada", [D, 6 * D], F32)
    b_ada_d = din("b_ada", [1, 6 * D], F32)
    g4_d = din("g4", [1, 4 * D], F32)
    w_in_d = din("w_in", [D, 1480], F32)
    gq_d = din("gq", [128, 2], F32)
    gkv_d = din("gkv", [128, 1], F32)
    w_q_b_d = din("w_q_b", [256, 768], F32)
    w_kv_b_d = din("w_kv_b", [128, 1024], F32)
    w_o_d = din("w_o", [D, D], F32)
    w_r_d = din("w_router", [D, NEXP], F32)
    b_r_d = din("b_router", [NEXP], F32)
    w1_d = din("w_mlp1", [NEXP, D, 2 * D], F32)
    b1_d = din("b_mlp1", [NEXP, 2 * D], F32)
    w2_d = din("w_mlp2", [NEXP, D, D], F32)
    b2_d = din("b_mlp2", [NEXP, D], F32)
    out_d = nc.dram_tensor("out", [S_LEN, D], F32, kind="ExternalOutput").ap()
    x1_d = nc.dram_tensor("x1s", [S_LEN, D], F32, kind="Internal").ap()
    dbg_out = {}

    with ExitStack() as es:
        S = Sched(nc, es)

        AW = 53000
        arena_t = es.enter_context(nc.sbuf_tensor("arena", [128, AW], F32))
        ast = {'off': 0, 'peak': 0}

        def _rel(m):
            ast['off'] = m

        def T(stack, name, shape, dt):
            P_ = shape[0]
            n = 1
            for d_ in shape[1:]:
                n *= d_
            esz = 4 if dt in (F32, I32) else 2
            words = (n * esz + 3) // 4
            words = (words + 7) // 8 * 8
            off = ast['off']
            assert off + words <= AW, "SBUF arena overflow at %s: %d + %d" % (name, off, words)
            stack.callback(_rel, off)
            ast['off'] = off + words
            ast['peak'] = max(ast['peak'], ast['off'])
            ap = arena_t[0:P_, off:off + words]
            if dt != F32:
                ap = ap.bitcast(dt)
            ap = ap[:, 0:n]
            if len(shape) > 2:
                names = ['a%d' % i for i in range(len(shape) - 1)]
                pat = "p (%s) -> p %s" % (' '.join(names), ' '.join(names))
                ap = ap.rearrange(pat, **{nm: sz for nm, sz in zip(names[:-1], shape[1:-1])})
            return ap

        PB = [es.enter_context(nc.psum_tensor("pb%d" % i, [128, 512], F32)) for i in range(7)]
        PTB = es.enter_context(nc.psum_tensor("ptb", [128, 1024], BF16))
        pbk = ['pb%d' % i for i in range(7)]

        def dump(name, ap, shape, dt=F32, reads=()):
            if not dbg:
                return
            t = nc.dram_tensor("dbg_" + name, list(shape), dt, kind="ExternalOutput").ap()
            dbg_out[name] = t
            ch = S.chan('dbg')
            S.dma('sp', ch, DMA(t, ap), reads=list(reads))

        ident_f = T(es, "ident_f", [128, 128], F32)
        ident_b = T(es, "ident_b", [128, 128], BF16)
        ones_f = T(es, "ones_f", [128, 128], F32)
        tri_b = T(es, "tri_b", [128, 128], BF16)
        COLS = T(es, "cols", [128, 32], F32)
        G1 = T(es, "g1", [128, D], F32)
        G2 = T(es, "g2", [128, D], F32)
        HT = T(es, "ht", [128, 8, S_LEN], BF16)
        small = T(es, "small", [128, 64], F32)
        epsc = T(es, "epsc", [128, 1], F32)
        hpi = T(es, "hpi", [128, 1], F32)
        S.op('pool', MS(hpi[:], math.pi / 2), writes=['hpi'])
        ZT = T(es, "zt", [128, D], BF16)
        S.op('pool', MS(ZT[:], 0.0), writes=['zt'])

        S.op('pool', MS(ident_f[:], 1.0), writes=['ident_f'])
        S.op('pool', lambda e: e.affine_select(out=ident_f[:], in_=ident_f[:], pattern=[[-1, 128]],
                                               compare_op=ALU.is_equal, fill=0.0, base=0, channel_multiplier=1),
             reads=['ident_f'], writes=['ident_f'])
        S.op('dve', CP(ident_b[:], ident_f[:]), reads=['ident_f'], writes=['ident_b'])
        S.op('pool', MS(ones_f[:], 1.0), writes=['ones_f'])
        S.op('pool', MS(epsc[:], EPS), writes=['epsc'])
        S.op('pool', MS(tri_b[:], 1.0), writes=['tri_b'])
        S.op('pool', lambda e: e.affine_select(out=tri_b[:], in_=tri_b[:], pattern=[[1, 128]],
                                               compare_op=ALU.is_ge, fill=0.0, base=0, channel_multiplier=-1),
             reads=['tri_b'], writes=['tri_b'])


        with ExitStack() as p0:
            CT = T(p0, "ct", [128, 8], F32)
            CS = T(p0, "cs", [128, 8], F32)
            MODR = T(p0, "modr", [1, 6 * D], F32)
            BADA = T(p0, "bada", [1, 6 * D], F32)
            GROW = T(p0, "grow", [1, 4 * D], F32)
            ROWS = T(p0, "rows", [1, 4 * D], F32)
            NWB = 4
            WAD = [T(p0, "wad%d" % i, [128, 8, 512], BF16) for i in range(NWB)]
            cw = [S.chan('wad%d' % i) for i in range(NWB)]
            CSB = T(p0, "csb", [128, 8], BF16)
            S.dma('sp', S.chan(), DMA(CT[:], c_d), writes=['ct'])
            S.dma('sp', S.chan(), DMA(BADA[:], b_ada_d), writes=['bada'])
            S.dma('sp', S.chan(), DMA(GROW[:], g4_d), writes=['grow'])
            S.op('act', ACTV(CS[:], CT[:], AF.Silu), reads=['ct'], writes=['cs'])
            S.op('dve', CP(CSB[:], CS[:]), reads=['cs'], writes=['csb'])
            wv = w_ada_d.rearrange("(k p) n -> p k n", p=128)
            for n in range(12):
                b = n % NWB
                pb_ = n % 2
                S.dma('pool', cw[b], DMA(WAD[b][:], wv[:, :, n * 512:(n + 1) * 512]), writes=['wad%d' % b])
                S.group('pe', [MM(PB[pb_][0:1, :], CSB[:, k:k + 1], WAD[b][:, k, :], k == 0, k == 7) for k in range(8)],
                        reads=['csb', 'wad%d' % b], writes=[pbk[pb_]])
                S.op('dve', TT(MODR[0:1, n * 512:(n + 1) * 512], PB[pb_][0:1, :], BADA[0:1, n * 512:(n + 1) * 512], ALU.add),
                     reads=[pbk[pb_], 'bada'], writes=['modr%d' % n])
            mk = lambda a, b_: ['modr%d' % n for n in range(a // 512, (b_ + 511) // 512)]
            sh1, sc1, gt1, sh2, sc2, gt2 = [(i * D, (i + 1) * D) for i in range(6)]
            S.op('dve', STT(ROWS[0:1, 0:D], MODR[0:1, sc1[0]:sc1[1]], 1.0, GROW[0:1, 0:D], ALU.add, ALU.mult),
                 reads=mk(*sc1) + ['grow'], writes=['rows0'])
            S.op('dve', TT(ROWS[0:1, D:2 * D], MODR[0:1, gt1[0]:gt1[1]], GROW[0:1, D:2 * D], ALU.mult),
                 reads=mk(*gt1) + ['grow'], writes=['rows1'])
            S.op('dve', STT(ROWS[0:1, 2 * D:3 * D], MODR[0:1, sc2[0]:sc2[1]], 1.0, GROW[0:1, 2 * D:3 * D], ALU.add, ALU.mult),
                 reads=mk(*sc2) + ['grow'], writes=['rows2'])
            S.op('dve', TT(ROWS[0:1, 3 * D:4 * D], MODR[0:1, gt2[0]:gt2[1]], GROW[0:1, 3 * D:4 * D], ALU.mult),
                 reads=mk(*gt2) + ['grow'], writes=['rows3'])
            srcs = [(ROWS, 0), (MODR, sh1[0]), (ROWS, 2 * D), (MODR, sh2[0])]
            fns = []
            for v, (tt_, off) in enumerate(srcs):
                for k in range(8):
                    fns.append(MM(PB[2][:, v * 8 + k:v * 8 + k + 1], tt_[0:1, off + k * 128:off + (k + 1) * 128],
                                  ones_f[0:1, 0:1]))
            S.group('pe', fns, reads=['rows0', 'rows2', 'ones_f'] + mk(*sh1) + mk(*sh2), writes=[pbk[2]])
            S.op('dve', CP(COLS[:], PB[2][:, 0:32]), reads=[pbk[2]], writes=['cols'])
            for gi, (G, off) in enumerate([(G1, D), (G2, 3 * D)]):
                for hf in range(2):
                    pbi = 3 + hf
                    S.group('pe', [MM(PB[pbi][:, :], ones_f[0:1, 0:128], ROWS[0:1, off + hf * 512:off + (hf + 1) * 512])],
                            reads=['rows1', 'rows3', 'ones_f'], writes=[pbk[pbi]])
                    S.op('act', ACTV(G[:, hf * 512:(hf + 1) * 512], PB[pbi][:, :], AF.Copy), reads=[pbk[pbi]],
                         writes=['g%d' % gi])
            dump("modr", MODR[:], [1, 6 * D], reads=mk(0, 6 * D))
            dump("cols", COLS[:], [128, 32], reads=['cols'])
            dump("g1", G1[:], [128, D], reads=['g0'])
            S.barrier()
        A1, B1c, A2, B2c = COLS[:, 0:8], COLS[:, 8:16], COLS[:, 16:24], COLS[:, 24:32]

        def norm_transpose(stk, load_tile, A, Bc, tag, dst, pre=None, post=None):
            XB = [T(stk, "%s_xb%d" % (tag, i), [128, D], F32) for i in range(4)]
            XN = [T(stk, "%s_xn%d" % (tag, i), [128, D], BF16) for i in range(4)]
            ST = T(stk, "%s_st" % tag, [128, 16], F32)
            for tc in range(NC4):
                for q in range(4):
                    tt = tc * 4 + q
                    load_tile(tt, XB[q], '%s_xb%d' % (tag, q))
                    S.op('act', ACTV(XN[q][:], XB[q][:], AF.Square, accum_out=ST[:, q:q + 1]),
                         reads=['%s_xb%d' % (tag, q)], writes=['%s_xn%d' % (tag, q), '%s_ss%d' % (tag, q)])
                S.op('act', ACTV(ST[:, 4:8], ST[:, 0:4], AF.Sqrt, scale=1.0 / D, bias=epsc[:, 0:1]),
                     reads=['%s_ss%d' % (tag, q) for q in range(4)] + ['epsc'], writes=['%s_sd' % tag])
                S.op('dve', lambda e: e.reciprocal(out=ST[:, 8:12], in_=ST[:, 4:8]), reads=['%s_sd' % tag],
                     writes=['%s_rs' % tag])
                for q in range(4):
                    S.op('dve', TS(XN[q][:], XB[q][:], ST[:, 8 + q:9 + q], None, ALU.mult),
                         reads=['%s_xb%d' % (tag, q), '%s_rs' % tag], writes=['%s_xn%d' % (tag, q)])
                    if pre is not None:
                        pre(tc * 4 + q, XN[q], '%s_xn%d' % (tag, q))
                for kp in range(4):
                    fns = []
                    for kk in range(2):
                        k = kp * 2 + kk
                        for q in range(4):
                            fns.append(TR(PTB[:, kk * 512 + q * 128:kk * 512 + (q + 1) * 128],
                                          XN[q][:, k * 128:(k + 1) * 128], ident_b[:]))
                    S.group('pe', fns, reads=['%s_xn%d' % (tag, q) for q in range(4)] + ['ident_b'], writes=['ptb'])
                    for kk in range(2):
                        k = kp * 2 + kk
                        if kk == 0:
                            S.op('act', ACTV(dst[:, k, tc * 512:(tc + 1) * 512], PTB[:, 0:512], AF.Identity,
                                             scale=A[:, k:k + 1], bias=Bc[:, k:k + 1]),
                                 reads=['ptb', 'cols'], writes=['%s_d%d' % (tag, tc)])
                        else:
                            S.op('dve', TS(dst[:, k, tc * 512:(tc + 1) * 512], PTB[:, 512:1024], A[:, k:k + 1],
                                           Bc[:, k:k + 1], ALU.mult, ALU.add),
                                 reads=['ptb', 'cols'], writes=['%s_d%d' % (tag, tc)])
                if post is not None:
                    post(tc)

        CAP = 512
        NSL = NEXP * CAP
        xs_d = nc.dram_tensor("xslots", [NSL, D], BF16, kind="Internal").ap()
        ys_d = nc.dram_tensor("yslots", [NSL, D], BF16, kind="Internal").ap()
        HTK = lambda tc: ['h_d%d' % tc]
        GATE = T(es, "gate", [128, NT, NEXP], F32)
        IDXS = T(es, "idxs", [128, NT, 4], I32)
        GK = T(es, "gk", [128, NT, 4], F32)
        rt6 = T(es, "rt6", [128, 64], F32)
        pmx = ExitStack()
        MIX = T(pmx, "mix", [128, 8, S_LEN], BF16)
        with ExitStack() as pa:
            cwt = S.chan('attw')
            WBt = T(pa, "wb", [128, 8, 776], BF16)
            FRQ = T(pa, "frq", [128, 1], F32)
            POSF = T(pa, "posf", [128, S_LEN], F32)
            PCI = T(pa, "pci", [128, NT], I32)
            NPC = T(pa, "npc", [128, NT], F32)
            KD = T(pa, "kd", [128, S_LEN], BF16)
            VD = T(pa, "vd", [128, NT, 2, 128], BF16)
            KI = T(pa, "ki", [96, S_LEN], BF16)
            GQ = T(pa, "gq", [128, 2], F32)
            GKV = T(pa, "gkv", [128, 1], F32)
            pm = ExitStack()
            WA = T(pm, "wa", [128, 8, 704], BF16)
            wiv = w_in_d.rearrange("(k p) n -> p k n", p=128)
            for (dst_, d0, s0, n_) in [(WA, 0, 0, 416), (WA, 416, 928, 256), (WA, 672, 1440, 32),
                                        (WBt, 512, 1184, 256), (WBt, 768, 1472, 8)]:
                S.dma('pool', cwt, DMA(dst_[:, :, d0:d0 + n_], wiv[:, :, s0:s0 + n_]), writes=['wa', 'wb'])
            wbq = WBt[:, :, 0:512].rearrange("p k (g kv d) -> p k g kv d", g=4, kv=2)
            for kv in range(2):
                for g in range(4):
                    c0_ = 416 + kv * 256 + g * 64
                    S.dma('pool', cwt, DMA(wbq[:, :, g, kv, :], wiv[:, :, c0_:c0_ + 64]), writes=['wa', 'wb'])
            WKI3 = T(pm, "wki3", [128, 8, 96], BF16)
            for r3 in range(3):
                S.op('pool', CP(WKI3[:, :, r3 * 32:(r3 + 1) * 32], WA[:, :, 672:704]), reads=['wa'], writes=['wki3'])
            WKR = T(pm, "wkr", [128, 8, 96], BF16)
            WKRS = T(pm, "wkrs", [128, 8, 96], BF16)
            S.op('pool', MS(WKR[:], 0.0), writes=['wkr'])
            S.op('pool', MS(WKRS[:], 0.0), writes=['wkrs'])
            S.op('pool', CP(WKR[:, :, 64:96], WA[:, :, 384:416]), reads=['wa'], writes=['wkr'])
            S.op('pool', TS(WKRS[:, :, 64:80], WA[:, :, 400:416], -1.0, None, ALU.mult), reads=['wa'], writes=['wkrs'])
            S.op('pool', CP(WKRS[:, :, 80:96], WA[:, :, 384:400]), reads=['wa'], writes=['wkrs'])
            WQB = T(pm, "wqb", [128, 2, 768], BF16)
            WQBS = T(pm, "wqbs", [128, 2, 768], BF16)
            WKVB = T(pm, "wkvb", [128, 1024], BF16)
            S.dma('pool', S.chan(), DMA(WQB[:], w_q_b_d.rearrange("(k p) n -> p k n", p=128)), writes=['wqb'])
            S.dma('pool', S.chan(), DMA(WKVB[:], w_kv_b_d), writes=['wkvb'])
            S.op('pool', MS(WQBS[:], 0.0), writes=['wqbs'])
            wq4 = WQB[:].rearrange("p k (h f) -> p k h f", h=8)
            wqs4 = WQBS[:].rearrange("p k (h f) -> p k h f", h=8)
            for m in range(2):
                S.op('pool', TS(wqs4[:, m, :, 64:80], wq4[:, m, :, 80:96], -1.0, None, ALU.mult), reads=['wqb'], writes=['wqbs'])
                S.op('pool', CP(wqs4[:, m, :, 80:96], wq4[:, m, :, 64:80]), reads=['wqb'], writes=['wqbs'])
            S.dma('sp', S.chan(), DMA(GQ[:], gq_d), writes=['gq'])
            S.dma('sp', S.chan(), DMA(GKV[:], gkv_d), writes=['gkv'])
            S.dma('sp', S.chan(), DMA(FRQ[:], frq_d), writes=['frq'])
            S.dma('sp', S.chan(), DMA(PCI[:], posc_d), writes=['pci'])
            S.op('dve', CP(NPC[:], PCI[:]), reads=['pci'], writes=['npc'])
            S.op('dve', TS(NPC[:], NPC[:], -1.0, None, ALU.mult), reads=['npc'], writes=['npc'])
            COS = T(pm, "cos", [128, S_LEN], F32)
            SIN = T(pm, "sin", [128, S_LEN], F32)
            pr = ExitStack()
            if True:
                POSI = T(pr, "posi", [128, S_LEN], I32)
                U = T(pr, "ru", [128, S_LEN], F32)
                UI = POSI
                U2 = T(pr, "ru2", [128, S_LEN], F32)
                S.dma('sp', S.chan(), DMA(POSI[:], posr_d.partition_broadcast(128)), writes=['posi'])
                S.op('dve', CP(POSF[:], POSI[:]), reads=['posi'], writes=['posf'])
                rs = slice(64, 96)
                S.op('dve', TS(U[rs, :], POSF[rs, :], FRQ[rs, 0:1], None, ALU.mult), reads=['posf', 'frq'], writes=['ru'])
                S.op('dve', CP(UI[rs, :], U[rs, :]), reads=['ru', 'posi'], writes=['posi'])
                S.op('dve', CP(U2[rs, :], UI[rs, :]), reads=['posi'], writes=['ru2'])
                S.op('dve', TT(U[rs, :], U[rs, :], U2[rs, :], ALU.subtract), reads=['ru', 'ru2'], writes=['ru'])
                S.op('dve', STT(U2[rs, :], U[rs, :], 0.5, U[rs, :], ALU.is_ge, ALU.subtract), reads=['ru'], writes=['ru2'])
                S.op('dve', STT(U[rs, :], U[rs, :], -0.5, U2[rs, :], ALU.is_lt, ALU.subtract), reads=['ru', 'ru2'], writes=['ru'])
                S.op('act', ACTV(SIN[rs, :], U[rs, :], AF.Sin, scale=2 * math.pi), reads=['ru'], writes=['sin'])
                S.op('act', ACTV(U2[rs, :], U[rs, :], AF.Abs, scale=2 * math.pi), reads=['ru'], writes=['ru2'])
                S.op('act', ACTV(COS[rs, :], U2[rs, :], AF.Sin, scale=-1.0, bias=hpi[rs, 0:1]), reads=['ru2', 'hpi'], writes=['cos'])
                dump("cos", COS[64:96, :], [32, S_LEN], reads=['cos'])
                dump("sin", SIN[64:96, :], [32, S_LEN], reads=['sin'])
            p1 = ExitStack()
            cx = [S.chan('x%d' % i) for i in range(4)]

            def load_x(tt, buf, key):
                S.dma('sp', cx[tt % 4], DMA(buf[:], x_d[tt * 128:(tt + 1) * 128, :]), writes=[key])
            norm_transpose(p1, load_x, A1, B1c, 'h', HT)
            dump("ht", HT[:], [128, 8, S_LEN], BF16, reads=['h_d%d' % i for i in range(4)])
            S.barrier()
            p1.close()
            pr.close()

            CQN = T(pm, "cqn", [128, 2, S_LEN], BF16)
            CKVN = T(pm, "ckvn", [128, S_LEN], BF16)
            KR = T(pm, "kr", [128, S_LEN], BF16)
            S.op('pool', MS(VD[:, :, :, 64:128], 1.0), writes=['vd'])

            with ExitStack() as pA:
                SQ = [T(pA, "sq%d" % i, [128, 512], F32) for i in range(3)]
                RQ = T(pA, "rq", [128, 512], F32)
                RKV = T(pA, "rkv", [128, 512], F32)
                TA = T(pA, "ta", [128, 512], F32)
                TB2 = T(pA, "tb2", [128, 512], F32)
                for tc in range(NC4):
                    ts_ = slice(tc * 512, (tc + 1) * 512)
                    hk = HTK(tc)
                    for m in range(3):
                        S.group('pe', [MM(PB[m][:, :], WA[:, k, m * 128:(m + 1) * 128], HT[:, k, ts_], k == 0, k == 7)
                                       for k in range(8)], reads=hk + ['wa'], writes=[pbk[m]])
                        S.op('act', ACTV(SQ[m][:], PB[m][:, :], AF.Square), reads=[pbk[m]], writes=['sq%d' % m])
                    S.group('pe', [MM(PB[3][:, :], ones_f[:, :], SQ[0][:], True, False),
                                   MM(PB[3][:, :], ones_f[:, :], SQ[1][:], False, True)],
                            reads=['sq0', 'sq1', 'ones_f'], writes=[pbk[3]])
                    S.group('pe', [MM(PB[4][:, :], ones_f[:, :], SQ[2][:], True, True)],
                            reads=['sq2', 'ones_f'], writes=[pbk[4]])
                    S.op('act', ACTV(RQ[:], PB[3][:, :], AF.Sqrt, scale=1.0 / 256, bias=epsc[:, 0:1]), reads=[pbk[3], 'epsc'], writes=['rq'])
                    S.op('act', ACTV(RKV[:], PB[4][:, :], AF.Sqrt, scale=1.0 / 128, bias=epsc[:, 0:1]), reads=[pbk[4], 'epsc'], writes=['rkv'])
                    S.op('dve', lambda e: e.reciprocal(out=RQ[:], in_=RQ[:]), reads=['rq'], writes=['rq'])
                    S.op('dve', lambda e: e.reciprocal(out=RKV[:], in_=RKV[:]), reads=['rkv'], writes=['rkv'])
                    for m in range(2):
                        S.op('dve', STT(CQN[:, m, ts_], PB[m][:, :], GQ[:, m:m + 1], RQ[:], ALU.mult, ALU.mult),
                             reads=[pbk[m], 'gq', 'rq'], writes=['cqn%d' % tc])
                    S.op('dve', STT(CKVN[:, ts_], PB[2][:, :], GKV[:, 0:1], RKV[:], ALU.mult, ALU.mult),
                         reads=[pbk[2], 'gkv', 'rkv'], writes=['ckvn%d' % tc])
                    S.group('pe', [MM(PB[5][0:96, :], WKR[:, k, :], HT[:, k, ts_], k == 0, k == 7) for k in range(8)],
                            reads=hk + ['wkr'], writes=[pbk[5]])
                    S.group('pe', [MM(PB[6][0:96, :], WKRS[:, k, :], HT[:, k, ts_], k == 0, k == 7) for k in range(8)],
                            reads=hk + ['wkrs'], writes=[pbk[6]])
                    S.op('dve', TT(TA[64:96, :], PB[5][64:96, :], COS[64:96, ts_], ALU.mult), reads=[pbk[5], 'cos'], writes=['ta'])
                    S.op('dve', TT(TB2[64:96, :], PB[6][64:96, :], SIN[64:96, ts_], ALU.mult), reads=[pbk[6], 'sin'], writes=['tb2'])
                    S.op('pool', TT(KR[64:96, ts_], TA[64:96, :], TB2[64:96, :], ALU.add), reads=['ta', 'tb2'], writes=['kr%d' % tc])
                    S.group('pe', [MM(PB[0][:, :], WA[:, k, 416:544], HT[:, k, ts_], k == 0, k == 7)
                                   for k in range(8)], reads=hk + ['wa'], writes=[pbk[0]])
                    S.op('act', ACTV(KD[:, ts_], PB[0][:, :], AF.Copy), reads=[pbk[0]], writes=['kd%d' % tc])
                    S.group('pe', [MM(PB[2][0:96, :], WKI3[:, k, :], HT[:, k, ts_], k == 0, k == 7) for k in range(8)],
                            reads=hk + ['wki3'], writes=[pbk[2]])
                    S.op('act', ACTV(KI[0:96, ts_], PB[2][0:96, :], AF.Copy), reads=[pbk[2]], writes=['ki%d' % tc])
                    fns = []
                    for q in range(4):
                        tt = tc * 4 + q
                        for k in range(8):
                            fns.append(MM(PB[3][:, q * 128:(q + 1) * 128], HT[:, k, tt * 128:(tt + 1) * 128], WA[:, k, 544:672],
                                          k == 0, k == 7))
                    S.group('pe', fns, reads=hk + ['wa'], writes=[pbk[3]])
                    S.op('act', ACTV(VD[:, tc * 4:(tc + 1) * 4, :, 0:64],
                                     PB[3][:, :].rearrange("p (q v d) -> p q v d", q=4, v=2), AF.Copy),
                         reads=[pbk[3]], writes=['vd'])
                dump("cqn", CQN[:], [128, 2, S_LEN], BF16, reads=['cqn%d' % i for i in range(4)])
                dump("ckvn", CKVN[:], [128, S_LEN], BF16, reads=['ckvn%d' % i for i in range(4)])
                dump("kr", KR[64:96, :], [32, S_LEN], BF16, reads=['kr%d' % i for i in range(4)])
                dump("vd", VD[:], [128, NT, 2, 128], BF16, reads=['vd'])
                S.barrier()
            if stage <= 2:
                S.finish(); S.replay()
                return nc, dbg_out

            with ExitStack() as pB:
                QT = [T(pB, "qt0", [96, S_LEN], BF16)] * 2
                KT = [T(pB, "kt0", [96, S_LEN], BF16)] * 2
                VA = [T(pB, "va0", [128, NT, 128], BF16)] * 2
                TA = T(pB, "mta", [128, 512], F32)
                TB2 = T(pB, "mtb", [128, 512], F32)
                PTl = [T(pB, "pt%d" % i, [128, 512], BF16) for i in range(3)]
                RT = [T(pB, "rt%d" % i, [64, 512], F32) for i in range(2)]
                S.op('pool', MS(VA[0][:, :, 64:128], 1.0), writes=['va0'])
                scale = (64 + 32) ** -0.5
                cz = S.chan('zero')
                S.hold = {cz}
                for a_ in range(NSL // 128):
                    S.dma('sp', cz, DMA(xs_d[a_ * 128:(a_ + 1) * 128, :], ZT[:]), reads=['zt'], writes=['xs_z%d' % a_])
                pti = 0
                for h in range(8):
                    hb = 0
                    qk, kk_, vk = 'qt%d' % hb, 'kt%d' % hb, 'va%d' % hb
                    for tc in range(NC4):
                        ts_ = slice(tc * 512, (tc + 1) * 512)
                        S.group('pe', [MM(PB[0][0:96, :], WQB[:, m, h * 96:(h + 1) * 96], CQN[:, m, ts_], m == 0, m == 1) for m in range(2)],
                                reads=['wqb', 'cqn%d' % tc], writes=[pbk[0]])
                        S.group('pe', [MM(PB[1][0:96, :], WQBS[:, m, h * 96:(h + 1) * 96], CQN[:, m, ts_], m == 0, m == 1) for m in range(2)],
                                reads=['wqbs', 'cqn%d' % tc], writes=[pbk[1]])
                        S.op('act', ACTV(QT[hb][0:64, ts_], PB[0][0:64, :], AF.Copy), reads=[pbk[0]], writes=[qk])
                        S.op('dve', TT(TA[64:96, :], PB[0][64:96, :], COS[64:96, ts_], ALU.mult), reads=[pbk[0], 'cos'], writes=['mta'])
                        S.op('dve', TT(TB2[64:96, :], PB[1][64:96, :], SIN[64:96, ts_], ALU.mult), reads=[pbk[1], 'sin'], writes=['mtb'])
                        S.op('pool', TT(QT[hb][64:96, ts_], TA[64:96, :], TB2[64:96, :], ALU.add), reads=['mta', 'mtb'], writes=[qk])
                        S.group('pe', [MM(PB[2][0:64, :], WKVB[:, h * 128:h * 128 + 64], CKVN[:, ts_])],
                                reads=['wkvb', 'ckvn%d' % tc], writes=[pbk[2]])
                        S.op('act', ACTV(KT[hb][0:64, ts_], PB[2][0:64, :], AF.Copy), reads=[pbk[2]], writes=[kk_])
                        S.op('pool', CP(KT[hb][64:96, ts_], KR[64:96, ts_]), reads=['kr%d' % tc], writes=[kk_])
                        S.group('pe', [MM(PB[2][:, q * 64:(q + 1) * 64], CKVN[:, (tc * 4 + q) * 128:(tc * 4 + q + 1) * 128],
                                          WKVB[:, h * 128 + 64:(h + 1) * 128]) for q in range(4)],
                                reads=['wkvb', 'ckvn%d' % tc], writes=[pbk[2]])
                        S.op('dve', CP(VA[hb][:, tc * 4:(tc + 1) * 4, 0:64], PB[2][:, 0:256].rearrange("p (q d) -> p q d", q=4)),
                             reads=[pbk[2]], writes=[vk])
                    if dbg and h == 0:
                        dump("qt0", QT[0][:], [96, S_LEN], BF16, reads=[qk])
                        dump("kt0", KT[0][:], [96, S_LEN], BF16, reads=[kk_])
                        dump("va0", VA[0][:], [128, NT, 128], BF16, reads=[vk])
                    blocks = [(c, j) for c in range(NC4) for j in range(4 * c + 4)]
                    LA = 3

                    def emit_score(bi, h=h, qk=qk, kk_=kk_):
                        c, j = blocks[bi]
                        c0 = max(0, j - 4 * c) * 128
                        ps = 2 + (bi % 3)
                        S.group('pe', [MM(PB[ps][:, c0:512], KT[0][0:96, j * 128:(j + 1) * 128],
                                          QT[0][0:96, c * 512 + c0:(c + 1) * 512])],
                                reads=[qk, kk_], writes=[pbk[ps]])
                    for bi in range(min(LA, len(blocks))):
                        emit_score(bi)
                    for bi, (c, j) in enumerate(blocks):
                        po = 5 + (c % 2)
                        nj = 4 * c + 4
                        r = j - 4 * c
                        c0 = max(0, r) * 128
                        ps = 2 + (bi % 3)
                        pt = PTl[bi % 3]
                        ptk = 'pt%d' % (bi % 3)
                        S.op('act', ACTV(pt[:, c0:512], PB[ps][:, c0:512], AF.Exp, scale=scale), reads=[pbk[ps]], writes=[ptk])
                        if r >= 0:
                            S.op('pool', TT(pt[:, c0:c0 + 128], pt[:, c0:c0 + 128], tri_b[:], ALU.mult),
                                 reads=[ptk, 'tri_b'], writes=[ptk])
                        S.group('pe', [MM(PB[po][:, c0:512], VA[0][:, j, :], pt[:, c0:512], j == 0, j == nj - 1)],
                                reads=[vk, ptk], writes=[pbk[po]])
                        if bi + LA < len(blocks):
                            emit_score(bi + LA)
                        if j == nj - 1:
                            rt = RT[c % 2]
                            rk = 'rt%d' % (c % 2)
                            S.op('dve', lambda e, rt=rt, po=po: e.reciprocal(out=rt[0:64, :], in_=PB[po][64:128, :]),
                                 reads=[pbk[po]], writes=[rk])
                            S.op('dve', TT(MIX[(h % 2) * 64:(h % 2) * 64 + 64, h // 2, c * 512:(c + 1) * 512],
                                           PB[po][0:64, :], rt[0:64, :], ALU.mult),
                                 reads=[pbk[po], rk], writes=['mix%d' % c])
                dump("mix_mla", MIX[:, 0:4, :], [128, 4, S_LEN], BF16, reads=['mix%d' % c for c in range(4)])
                S.barrier()
            pm.close()
            if stage <= 3:
                S.finish(); S.replay()
                return nc, dbg_out

            with ExitStack() as pC:
                QD = T(pC, "qd", [128, 4, 512], BF16)
                QI = T(pC, "qi", [96, 3, 512], BF16)
                WI = T(pC, "wi", [128, 4, 8], F32)
                IDX2 = [T(pC, "idx%d" % i, [128, S_LEN], F32) for i in range(2)]
                MID = T(pC, "mid", [128, 2], F32)
                CNT = T(pC, "cnt", [128, 2], F32)
                TQ = T(pC, "tq", [128, 2], F32)
                RL = [T(pC, "rl%d" % i, [128, 512], F32) for i in range(2)]
                M01 = [T(pC, "m01_%d" % i, [128, S_LEN], BF16) for i in range(2)]
                MT2 = [T(pC, "mt%d" % i, [128, NT, 512], BF16) for i in range(2)]
                THR = T(pC, "thr", [128, 2], F32)
                DT = [T(pC, "dt%d" % i, [128, 512], F32) for i in range(3)]
                EE = [T(pC, "ee%d" % i, [128, 512], BF16) for i in range(3)]
                PM = [T(pC, "pm%d" % i, [128, 512], BF16) for i in range(3)]
                RT = [T(pC, "drt%d" % i, [64, 512], F32) for i in range(2)]
                SLD = T(pC, "sld", [128, 8, 128], F32)
                for hd in range(8):
                    S.op('pool', TS(SLD[:, hd, :], ident_f[:], -8.0 * 2.0 ** (-(hd + 1)), None, ALU.mult),
                         reads=['ident_f'], writes=['sld'])
                rlc = [0]

                def stage1_proj(c):
                    ts_ = slice(c * 512, (c + 1) * 512)
                    hk = HTK(c)
                    for slot in range(3):
                        nh = 3 if slot < 2 else 2
                        S.group('pe', [MM(PB[2][0:nh * 32, :], WBt[:, k, 512 + slot * 96:512 + slot * 96 + nh * 32], HT[:, k, ts_], k == 0, k == 7)
                                       for k in range(8)], reads=hk + ['wb'], writes=[pbk[2]])
                        S.op('dve', TS(QI[0:nh * 32, slot, :], PB[2][0:nh * 32, :], 32 ** -0.5, None, ALU.mult), reads=[pbk[2]], writes=['qi'])
                    fns = []
                    for q in range(4):
                        tt = c * 4 + q
                        for k in range(8):
                            fns.append(MM(PB[2][:, q * 8:(q + 1) * 8], HT[:, k, tt * 128:(tt + 1) * 128], WBt[:, k, 768:776], k == 0, k == 7))
                    S.group('pe', fns, reads=hk + ['wb'], writes=[pbk[2]])
                    S.op('dve', TS(WI[:].rearrange("p q h -> p (q h)"), PB[2][:, 0:32], 8 ** -0.5, None, ALU.mult),
                         reads=[pbk[2]], writes=['wi'])

                def stage1_idx(c, q0):
                    for t_ in range(2):
                        q = q0 + t_
                        i = c * 4 + q
                        ncols = (i + 1) * 128
                        nsc = (ncols + 511) // 512
                        IDX = IDX2[t_]
                        ik = 'idx%d' % t_
                        for hd in range(8):
                            grp, slot = hd % 3, hd // 3
                            for sc in range(nsc):
                                w = min(512, ncols - sc * 512)
                                pi = rlc[0] % 2
                                rl = RL[pi]
                                rlk = 'rl%d' % pi
                                rlc[0] += 1
                                S.group('pe', [MM(PB[pi][:, 0:w], QI[grp * 32:(grp + 1) * 32, slot, q * 128:(q + 1) * 128],
                                                  KI[grp * 32:(grp + 1) * 32, sc * 512:sc * 512 + w])],
                                        reads=['qi'] + ['ki%d' % sc], writes=[pbk[pi]])
                                S.op('act', ACTV(rl[:, 0:w], PB[pi][:, 0:w], AF.Relu), reads=[pbk[pi]], writes=[rlk])
                                if hd == 0:
                                    S.op('dve', TS(IDX[:, sc * 512:sc * 512 + w], rl[:, 0:w], WI[:, q, 0:1], None, ALU.mult),
                                         reads=[rlk, 'wi'], writes=[ik])
                                else:
                                    S.op('dve', STT(IDX[:, sc * 512:sc * 512 + w], rl[:, 0:w], WI[:, q, hd:hd + 1],
                                                    IDX[:, sc * 512:sc * 512 + w], ALU.mult, ALU.add),
                                         reads=[rlk, 'wi', ik], writes=[ik])
                        S.op('pool', lambda e, i=i, IDX=IDX: e.affine_select(out=IDX[:, i * 128:(i + 1) * 128], in_=IDX[:, i * 128:(i + 1) * 128],
                                                                 pattern=[[-1, 128]], compare_op=ALU.is_ge, fill=-1e30,
                                                                 base=0, channel_multiplier=1),
                             reads=[ik], writes=[ik])

                def stage1_bisect(c, q0):
                    i0 = c * 4 + q0
                    if i0 >= 2:
                        S.op('dve', MS(MID[:, 0:2], 0.0), writes=['mid'])
                        NIT = 26
                        for it in range(NIT):
                            w_it = 64.0 / (2.0 ** it)
                            for t_ in range(2):
                                ncols = (i0 + t_ + 1) * 128
                                S.op('dve', lambda e, t_=t_, ncols=ncols: e.tensor_scalar(
                                    out=M01[t_][:, 0:ncols], in0=IDX2[t_][:, 0:ncols], scalar1=MID[:, t_:t_ + 1], scalar2=0.0,
                                    op0=ALU.is_ge, op1=ALU.add, accum_out=CNT[:, t_:t_ + 1]),
                                    reads=['idx%d' % t_, 'mid'], writes=['m01_%d' % t_, 'cnt%d' % t_])
                            S.op('dve', TS(TQ[:, 0:2], CNT[:, 0:2], 256.0, w_it, ALU.is_ge, ALU.mult), reads=['cnt0', 'cnt1'], writes=['tq'])
                            if it < NIT - 1:
                                S.op('dve', STT(MID[:, 0:2], TQ[:, 0:2], -w_it / 2, MID[:, 0:2], ALU.add, ALU.add), reads=['tq', 'mid'], writes=['mid'])
                            else:
                                S.op('dve', STT(THR[:, 0:2], TQ[:, 0:2], -w_it, MID[:, 0:2], ALU.add, ALU.add), reads=['tq', 'mid'], writes=['thr'])
                    else:
                        S.op('dve', MS(THR[:, 0:2], -1e29), writes=['thr'])
                    for t_ in range(2):
                        ncols = (i0 + t_ + 1) * 128
                        S.op('dve', TS(M01[t_][:, 0:ncols], IDX2[t_][:, 0:ncols], THR[:, t_:t_ + 1], None, ALU.is_ge),
                             reads=['idx%d' % t_, 'thr'], writes=['m01_%d' % t_])

                def stage1_tr(c, q0):
                    MT = MT2[c % 2]
                    mk_ = 'mt%d' % (c % 2)
                    nj = 4 * c + 4
                    for t_ in range(2):
                        q = q0 + t_
                        i = c * 4 + q
                        for j0 in range(0, i + 1, 8):
                            jn = min(8, i + 1 - j0)
                            S.group('pe', [TR(PTB[:, jj * 128:(jj + 1) * 128], M01[t_][:, (j0 + jj) * 128:(j0 + jj + 1) * 128], ident_b[:])
                                           for jj in range(jn)], reads=['m01_%d' % t_, 'ident_b'], writes=['ptb'])
                            S.op('act', ACTV(MT[:, j0:j0 + jn, q * 128:(q + 1) * 128],
                                             PTB[:, 0:jn * 128].rearrange("p (j t) -> p j t", j=jn), AF.Copy),
                                 reads=['ptb'], writes=[mk_])
                        if i + 1 < nj:
                            S.op('pool', MS(MT[:, i + 1:nj, q * 128:(q + 1) * 128], 0.0), writes=[mk_])

                def stage2_proj(c):
                    ts_ = slice(c * 512, (c + 1) * 512)
                    hk = HTK(c)
                    for g in range(4):
                        pq = g % 2
                        S.group('pe', [MM(PB[pq][:, :], WBt[:, k, g * 128:(g + 1) * 128], HT[:, k, ts_], k == 0, k == 7) for k in range(8)],
                                reads=hk + ['wb'], writes=[pbk[pq]])
                        S.op('act', ACTV(QD[:, g, :], PB[pq][:, :], AF.Copy), reads=[pbk[pq]], writes=['qd'])

                bic = [0]

                def stage2_heads(c, heads):
                    ts_ = slice(c * 512, (c + 1) * 512)
                    MT = MT2[c % 2]
                    mk_ = 'mt%d' % (c % 2)
                    nj = 4 * c + 4
                    blocks = [(hd, j) for hd in heads for j in range(nj)]
                    LA = 3
                    base = bic[0]
                    bic[0] += len(blocks)

                    def emit_score(bi):
                        hd, j = blocks[bi]
                        kv, g = hd // 4, hd % 4
                        ps = 2 + ((base + bi) % 3)
                        d = (base + bi) % 3
                        S.op('act', ACTV(DT[d][:], POSF[:, ts_], AF.Abs, bias=NPC[:, j:j + 1]), reads=['posf', 'npc'], writes=['dt%d' % d])
                        S.group('pe', [MM(PB[ps][:, :], KD[kv * 64:(kv + 1) * 64, j * 128:(j + 1) * 128], QD[kv * 64:(kv + 1) * 64, g, :], True, False),
                                       MM(PB[ps][:, :], SLD[:, hd, :], DT[d][:], False, True)],
                                reads=['kd%d' % (j // 4), 'qd', 'sld', 'dt%d' % d], writes=[pbk[ps]])
                    for bi in range(min(LA, len(blocks))):
                        emit_score(bi)
                    for bi, (hd, j) in enumerate(blocks):
                        kv = hd // 4
                        po = 5 + (hd % 2)
                        ps = 2 + ((base + bi) % 3)
                        d = (base + bi) % 3
                        S.op('act', ACTV(EE[d][:], PB[ps][:, :], AF.Exp, scale=0.125), reads=[pbk[ps]], writes=['ee%d' % d])
                        S.op('pool', TT(PM[d][:], EE[d][:], MT[:, j, :], ALU.mult), reads=['ee%d' % d, mk_], writes=['pm%d' % d])
                        S.group('pe', [MM(PB[po][:, :], VD[:, j, kv, :], PM[d][:], j == 0, j == nj - 1)],
                                reads=['vd', 'pm%d' % d], writes=[pbk[po]])
                        if bi + LA < len(blocks):
                            emit_score(bi + LA)
                        if j == nj - 1:
                            rt = RT[hd % 2]
                            rk = 'drt%d' % (hd % 2)
                            S.op('dve', TS(rt[0:64, :], PB[po][64:128, :], 1e-30, None, ALU.max), reads=[pbk[po]], writes=[rk])
                            S.op('dve', lambda e, rt=rt: e.reciprocal(out=rt[0:64, :], in_=rt[0:64, :]), reads=[rk], writes=[rk])
                            S.op('dve', TT(MIX[(hd % 2) * 64:(hd % 2) * 64 + 64, 4 + hd // 2, ts_], PB[po][0:64, :], rt[0:64, :], ALU.mult),
                                 reads=[pbk[po], rk], writes=['mix%d' % c])

                stage1_proj(0)
                for q0 in (0, 2):
                    stage1_idx(0, q0)
                    stage1_bisect(0, q0)
                    stage1_tr(0, q0)
                for c in range(NC4):
                    stage2_proj(c)
                    nxt = c + 1 < NC4
                    if nxt:
                        stage1_proj(c + 1)
                        stage1_idx(c + 1, 0)
                        stage1_bisect(c + 1, 0)
                    stage2_heads(c, [0, 1, 2, 3])
                    if nxt:
                        stage1_tr(c + 1, 0)
                        stage1_idx(c + 1, 2)
                        stage1_bisect(c + 1, 2)
                    stage2_heads(c, [4, 5, 6, 7])
                    if nxt:
                        stage1_tr(c + 1, 2)
                dump("mix_all", MIX[:], [128, 8, S_LEN], BF16, reads=['mix%d' % c for c in range(4)])
                S.barrier()
        if stage <= 4:
            S.finish(); S.replay()
            return nc, dbg_out

        regc = {}

        def breg(e):
            if 'r' not in regc:
                regc['r'] = e.to_reg(NEXP * CAP - 1)
            return regc['r']
        BIG = float(NSL + 1)
        ph2 = ExitStack()
        AB2 = T(ph2, "ab2", [128, D], F32)
        BB2 = T(ph2, "bb2", [128, D], F32)
        H2TM = T(ph2, "h2tm", [128, NT, D], BF16)
        with ExitStack() as p5:
            WO = T(p5, "wo", [128, 8, D], BF16)
            cwo = S.chan('wo')
            S.dma('pool', cwo, DMA(WO[:], w_o_d.rearrange("(k p) n -> p k n", p=128)), writes=['wo'])
            BT = [T(p5, "bt%d" % i, [128, 128], F32) for i in range(2)]
            for vi, (colv, dstv) in enumerate([(A2, AB2), (B2c, BB2)]):
                for k in range(8):
                    bi = (vi * 8 + k) % 2
                    S.op('dve', TS(BT[bi][:], ones_f[:], colv[:, k:k + 1], None, ALU.mult), reads=['ones_f', 'cols'], writes=['bt%d' % bi])
                    S.group('pe', [MM(PB[4 + bi][:, 0:128], BT[bi][:], ident_f[:])], reads=['bt%d' % bi, 'ident_f'], writes=[pbk[4 + bi]])
                    S.op('act', ACTV(dstv[:, k * 128:(k + 1) * 128], PB[4 + bi][:, 0:128], AF.Copy), reads=[pbk[4 + bi]], writes=['abb%d' % vi])
            XR = [T(p5, "xr%d" % i, [128, D], F32) for i in range(2)]
            TM = [T(p5, "tm%d" % i, [128, D], F32) for i in range(2)]
            JK = T(p5, "jk", [128, 512], BF16)
            cxr = [S.chan('xr0'), S.chan('xr1')]
            cx1 = [S.chan('x1st%d' % i) for i in range(4)]
            st5 = T(p5, "st5", [128, 8], F32)

            def load_x1(tt, buf, key):
                b = tt % 2
                S.dma('sp', cxr[b], DMA(XR[b][:], x_d[tt * 128:(tt + 1) * 128, :]), writes=['xr%d' % b])
                for hf in range(2):
                    pbi = 2 * b + hf
                    S.group('pe', [MM(PB[pbi][:, :], MIX[:, k, tt * 128:(tt + 1) * 128], WO[:, k, hf * 512:(hf + 1) * 512], k == 0, k == 7)
                                   for k in range(8)], reads=['wo', 'mix%d' % (tt // 4)], writes=[pbk[pbi]])
                    S.op('act', ACTV(JK[:], PB[pbi][:, :], AF.Square, accum_out=st5[:, hf:hf + 1]),
                         reads=[pbk[pbi]], writes=['jk', 'st5_%d' % hf])
                S.op('dve', TT(st5[:, 2:3], st5[:, 0:1], st5[:, 1:2], ALU.add), reads=['st5_0', 'st5_1'], writes=['st5s'])
                S.op('act', ACTV(st5[:, 3:4], st5[:, 2:3], AF.Sqrt, scale=1.0 / D, bias=epsc[:, 0:1]), reads=['st5s', 'epsc'], writes=['st5q'])
                S.op('dve', lambda e: e.reciprocal(out=st5[:, 4:5], in_=st5[:, 3:4]), reads=['st5q'], writes=['st5r'])
                for hf in range(2):
                    pbi = 2 * b + hf
                    S.op('dve', STT(TM[b][:, hf * 512:(hf + 1) * 512], PB[pbi][:, :], st5[:, 4:5], G1[:, hf * 512:(hf + 1) * 512],
                                    ALU.mult, ALU.mult), reads=[pbk[pbi], 'st5r', 'g0'], writes=['tm%d' % b])
                S.op('dve', TT(buf[:], TM[b][:], XR[b][:], ALU.add), reads=['tm%d' % b, 'xr%d' % b], writes=[key])
                S.dma('sp', cx1[tt % 4], DMA(x1_d[tt * 128:(tt + 1) * 128, :], buf[:]), reads=[key])

            def mk_h2tm(tt, xn, key):
                b = tt % 2
                S.op('dve', TT(TM[b][:], xn[:], AB2[:], ALU.mult), reads=[key, 'abb0', 'tm%d' % b], writes=['tm%d' % b])
                S.op('dve', TT(H2TM[:, tt, :], TM[b][:], BB2[:], ALU.add), reads=['tm%d' % b, 'abb1'], writes=['h2tm'])
            p6s = p5
            WR = T(p6s, "wr", [128, 8, NEXP], BF16)
            BR = T(p6s, "br", [128, NEXP], F32)
            LG = T(p6s, "lg", [128, NEXP], F32)
            EX = T(p6s, "ex", [128, NEXP], F32)
            MK = T(p6s, "mk", [128, NEXP], F32)
            MKB = T(p6s, "mkb", [128, NT, NEXP], BF16)
            ones_b = T(p6s, "ones_b", [128, 128], BF16)
            tris_b = T(p6s, "tris_b", [128, 128], BF16)
            CUM = T(p6s, "cum", [128, NT, NEXP], F32)
            VAL = T(p6s, "val", [128, NT, NEXP], F32)
            KEY = T(p6s, "key", [128, NT, NEXP], F32)
            EOI = T(p6s, "eoi", [128, NT, NEXP], I32)
            EOF_ = T(p6s, "eof", [128, NT, NEXP], F32)
            K8 = T(p6s, "k8", [128, NT, 8], F32)
            SLF = T(p6s, "slf", [128, NT, 4], F32)
            JK6 = T(p6s, "jk6", [128, NEXP], F32)
            S.op('pool', MS(ones_b[:], 1.0), writes=['ones_b'])
            S.op('pool', MS(tris_b[:], 1.0), writes=['tris_b'])
            S.op('pool', lambda e: e.affine_select(out=tris_b[:], in_=tris_b[:], pattern=[[1, 128]],
                                                   compare_op=ALU.is_gt, fill=0.0, base=0, channel_multiplier=-1),
                 reads=['tris_b'], writes=['tris_b'])
            S.op('pool', lambda e: e.iota(EOI[:], pattern=[[0, NT], [CAP, NEXP]], base=0, channel_multiplier=0),
                 writes=['eoi'])
            S.op('dve', CP(EOF_[:], EOI[:]), reads=['eoi'], writes=['eof'])
            S.dma('pool', S.chan(), DMA(WR[:], w_r_d.rearrange("(k p) n -> p k n", p=128)), writes=['wr'])
            S.dma('sp', S.chan(), DMA(BR[:], b_r_d.partition_broadcast(128)), writes=['br'])
            cscl = [S.chan('scat%d' % i) for i in range(8)]
            S.hold = set()
            S._emit_waits('pool', {cz: S.count[cz]})

            def dispatch_chunk(tc):
                c4 = slice(tc * 4, (tc + 1) * 4)
                for tt in range(tc * 4, tc * 4 + 4):
                    S.group('pe', [MM(PB[5][:, 0:32], HT[:, k, tt * 128:(tt + 1) * 128], WR[:, k, :], k == 0, k == 7) for k in range(8)],
                            reads=['wr', 'g_d%d' % tc], writes=[pbk[5]])
                    S.op('dve', TT(LG[:], PB[5][:, 0:32], BR[:], ALU.add), reads=[pbk[5], 'br'], writes=['lg'])
                    S.op('dve', lambda e: e.max(out=rt6[:, 0:8], in_=LG[:]), reads=['lg'], writes=['r6a'])
                    S.op('dve', TS(rt6[:, 8:9], rt6[:, 0:1], -1.0, None, ALU.mult), reads=['r6a'], writes=['r6b'])
                    S.op('act', ACTV(EX[:], LG[:], AF.Exp, bias=rt6[:, 8:9]), reads=['lg', 'r6b'], writes=['ex'])
                    S.op('dve', TS(MK[:], LG[:], rt6[:, 3:4], None, ALU.is_ge), reads=['lg', 'r6a'], writes=['mk'])
                    S.op('dve', CP(MKB[:, tt, :], MK[:]), reads=['mk'], writes=['mkb'])
                    S.op('dve', STT(EX[:], EX[:], 1.0, MK[:], ALU.mult, ALU.mult, accum_out=rt6[:, 9:10]),
                         reads=['ex', 'mk'], writes=['ex', 'r6c'])
                    S.op('dve', lambda e: e.reciprocal(out=rt6[:, 10:11], in_=rt6[:, 9:10]), reads=['r6c'], writes=['r6d'])
                    S.op('dve', TS(GATE[:, tt, :], EX[:], rt6[:, 10:11], None, ALU.mult), reads=['ex', 'r6d'], writes=['gate'])
                for tt in range(tc * 4, tc * 4 + 4):
                    fns = [MM(PB[6][:, tt * 32:(tt + 1) * 32], ones_b[:], MKB[:, t2, :], t2 == 0, False) for t2 in range(tt)]
                    fns.append(MM(PB[6][:, tt * 32:(tt + 1) * 32], tris_b[:], MKB[:, tt, :], tt == 0, True))
                    S.group('pe', fns, reads=['mkb', 'ones_b', 'tris_b'], writes=[pbk[6]])
                pv = PB[6][:, tc * 128:(tc + 1) * 128].rearrange("p (a e) -> p a e", a=4)
                S.op('dve', CP(CUM[:, c4, :], pv), reads=[pbk[6]], writes=['cum'])
                S.op('dve', TS(VAL[:, c4, :], CUM[:, c4, :], float(CAP), None, ALU.is_lt), reads=['cum'], writes=['val'])
                S.op('dve', TT(GATE[:, c4, :], GATE[:, c4, :], VAL[:, c4, :], ALU.mult), reads=['gate', 'val'], writes=['gate'])
                S.op('dve', TT(KEY[:, c4, :], CUM[:, c4, :], EOF_[:, c4, :], ALU.add), reads=['cum', 'eof'], writes=['key'])
                S.op('dve', TS(KEY[:, c4, :], KEY[:, c4, :], -1.0, BIG, ALU.mult, ALU.add), reads=['key'], writes=['key'])
                S.op('dve', TS(VAL[:, c4, :], GATE[:, c4, :], 0.0, None, ALU.is_gt), reads=['gate'], writes=['val'])
                S.op('dve', TT(KEY[:, c4, :], KEY[:, c4, :], VAL[:, c4, :], ALU.mult), reads=['key', 'val'], writes=['key'])
                for tt in range(tc * 4, tc * 4 + 4):
                    S.op('dve', lambda e, tt=tt: e.max(out=K8[:, tt, :], in_=KEY[:, tt, :]), reads=['key'], writes=['k8'])
                S.op('dve', TS(SLF[:, c4, :], K8[:, c4, 0:4], -1.0, BIG, ALU.mult, ALU.add), reads=['k8'], writes=['slf'])
                S.op('dve', CP(IDXS[:, c4, :], SLF[:, c4, :]), reads=['slf'], writes=['idxs'])
                for tt in range(tc * 4, tc * 4 + 4):
                    for k in range(4):
                        S.op('dve', STT(JK6[:], KEY[:, tt, :], K8[:, tt, k:k + 1], GATE[:, tt, :], ALU.is_equal, ALU.mult,
                                        accum_out=GK[:, tt, k:k + 1]), reads=['key', 'k8', 'gate', 'jk6'], writes=['jk6', 'gk%d_%d' % (tt, k)])
                for tt in range(tc * 4, tc * 4 + 4):
                    for k in range(4):
                        S.dma('pool', cscl[(tt * 4 + k) % 8], lambda e, tt=tt, k=k: e.indirect_dma_start(
                            out=xs_d[:, :], out_offset=bass.IndirectOffsetOnAxis(ap=IDXS[:, tt, k:k + 1], axis=0),
                            in_=H2TM[:, tt, :], in_offset=None, bounds_check=breg(e), oob_is_err=False),
                            reads=['xs', 'idxs', 'h2tm'], writes=['xs_sc%d_%d' % (tt, k)], drain=True)
            norm_transpose(p5, load_x1, A2, B2c, 'g', HT, pre=mk_h2tm, post=dispatch_chunk)
            dump("h2t", HT[:], [128, 8, S_LEN], BF16, reads=['g_d%d' % i for i in range(4)])
            S.barrier()
        if stage <= 5:
            S.finish(); S.replay()
            return nc, dbg_out

        ph2.close()
        pmx.close()

        n_exp = NEXP if stage >= 7 else 2
        NSLOT = 6
        w1v = w1_d.rearrange("e (k p) n -> e p k n", p=128)
        w2v = w2_d.rearrange("e (k p) n -> e p k n", p=128)
        pieces = []
        for e_ in range(n_exp):
            for piece in range(4):
                pieces.append((e_, 0, piece))
            for dh in range(2):
                pieces.append((e_, 1, dh))
        with ExitStack() as p6e:
            WS = [T(p6e, "ws%d" % i, [128, 8, 512], BF16) for i in range(NSLOT)]
            cws = [S.chan('ws%d' % i) for i in range(NSLOT)]

            def issue(pi):
                if pi >= len(pieces):
                    return
                e_, kind, idx = pieces[pi]
                sl = pi % NSLOT
                src = (w1v if kind == 0 else w2v)[e_, :, :, idx * 512:(idx + 1) * 512]
                S.dma('pool', cws[sl], DMA(WS[sl][:], src), writes=['ws%d' % sl])
            for pi in range(NSLOT - 1):
                issue(pi)
            B1R = T(p6e, "b1r", [32, 2 * D], F32)
            B1C = T(p6e, "b1c", [128, 16, NEXP], F32)
            B1P = T(p6e, "b1p", [128, 16, NEXP], F32)
            S.dma('sp', S.chan(), DMA(B1R[:], b1_d), writes=['b1r'])
            b1v = B1R[:].rearrange("e (fc p two) -> e fc two p", fc=8, two=2)
            fns = []
            for fc in range(8):
                for two in range(2):
                    s_ = fc * 2 + two
                    fns.append(TR(PB[6][:, s_ * 32:(s_ + 1) * 32], b1v[:, fc, two, :], ident_f[0:32, 0:32]))
            S.group('pe', fns, reads=['b1r', 'ident_f'], writes=[pbk[6]])
            S.op('dve', CP(B1C[:].rearrange("p s e -> p (s e)"), PB[6][:, :]), reads=[pbk[6]], writes=['b1c'])
            S.op('dve', TS(B1P[:].rearrange("p s e -> p (s e)"), PB[6][:, :], 1.0, None, ALU.add), reads=[pbk[6]], writes=['b1c'])
            XE = [T(p6e, "xe%d" % i, [128, 4, D], BF16) for i in range(2)]
            XT = [T(p6e, "xt%d" % i, [128, 8, CAP], BF16) for i in range(2)]
            cxe = [S.chan('xe0'), S.chan('xe1')]
            GG = [T(p6e, "gg%d" % i, [128, 512], F32) for i in range(2)]
            SG = [T(p6e, "sg%d" % i, [128, 512], F32) for i in range(2)]
            LL = [T(p6e, "ll%d" % i, [128, 512], F32) for i in range(2)]
            ACT2 = [T(p6e, "act2_%d" % i, [128, 8, CAP], BF16) for i in range(2)]
            YB = [T(p6e, "yb%d" % i, [128, 4, D], BF16) for i in range(2)]
            cyb = [S.chan('yb0'), S.chan('yb1')]

            def load_xe(e_):
                if e_ >= n_exp:
                    return
                b = e_ % 2
                S.dma('sp', cxe[b], DMA(XE[b][:], xs_d[e_ * CAP:(e_ + 1) * CAP, :].rearrange("(a p) n -> p a n", p=128)),
                      writes=['xe%d' % b])

            def transposes(e_):
                if e_ >= n_exp:
                    return
                b = e_ % 2
                for st in range(4):
                    tb_, tk_ = (PTB[:, :], 'ptb') if st % 2 == 0 else (PB[6][:, :].bitcast(BF16), pbk[6])
                    S.group('pe', [TR(tb_[:, k * 128:(k + 1) * 128], XE[b][:, st, k * 128:(k + 1) * 128], ident_b[:]) for k in range(8)],
                            reads=['xe%d' % b, 'ident_b'], writes=[tk_])
                    if st % 2 == 0:
                        S.op('act', ACTV(XT[b][:, :, st * 128:(st + 1) * 128], tb_.rearrange("p (k s) -> p k s", k=8), AF.Copy),
                             reads=[tk_], writes=['xt%d' % b])
                    else:
                        S.op('act', ACTV(XT[b][:, :, st * 128:(st + 1) * 128], tb_.rearrange("p (k s) -> p k s", k=8), AF.Copy),
                             reads=[tk_], writes=['xt%d' % b])
            load_xe(0)
            load_xe(1)
            transposes(0)
            it = 0
            yi = 0
            pi = 0
            for e_ in range(n_exp):
                b = e_ % 2
                for piece in range(4):
                    sl = pi % NSLOT
                    issue(pi + NSLOT - 1)
                    pi += 1
                    for fcl in range(2):
                        fc = piece * 2 + fcl
                        bb = it % 2
                        it += 1
                        pg, pl = 2 * bb, 2 * bb + 1
                        S.group('pe', [MM(PB[pg][:, :], WS[sl][:, k, fcl * 256:fcl * 256 + 256:2], XT[b][:, k, :], k == 0, k == 7) for k in range(8)],
                                reads=['ws%d' % sl, 'xt%d' % b], writes=[pbk[pg]])
                        S.group('pe', [MM(PB[pl][:, :], WS[sl][:, k, fcl * 256 + 1:fcl * 256 + 256:2], XT[b][:, k, :], k == 0, k == 7) for k in range(8)],
                                reads=['ws%d' % sl, 'xt%d' % b], writes=[pbk[pl]])
                        S.op('dve', TS(GG[bb][:], PB[pg][:, :], B1C[:, fc * 2, e_:e_ + 1], 7.0, ALU.add, ALU.min),
                             reads=[pbk[pg], 'b1c'], writes=['gg%d' % bb])
                        S.op('act', ACTV(SG[bb][:], GG[bb][:], AF.Silu, scale=1.702), reads=['gg%d' % bb], writes=['sg%d' % bb])
                        S.op('act', ACTV(LL[bb][:], PB[pl][:, :], AF.Identity, bias=B1P[:, fc * 2 + 1, e_:e_ + 1]),
                             reads=[pbk[pl], 'b1c'], writes=['ll%d' % bb])
                        S.op('dve', TS(LL[bb][:], LL[bb][:], 8.0, -6.0, ALU.min, ALU.max), reads=['ll%d' % bb], writes=['ll%d' % bb])
                        S.op('dve', STT(ACT2[b][:, fc, :], SG[bb][:], 1.0 / 1.702, LL[bb][:], ALU.mult, ALU.mult),
                             reads=['sg%d' % bb, 'll%d' % bb], writes=['act2_%d' % b])
                transposes(e_ + 1)
                for dh in range(2):
                    sl = pi % NSLOT
                    issue(pi + NSLOT - 1)
                    pi += 1
                    for st in range(4):
                        py = 4 + (yi % 2)
                        yi += 1
                        S.group('pe', [MM(PB[py][:, :], ACT2[b][:, f, st * 128:(st + 1) * 128], WS[sl][:, f, :], f == 0, f == 7) for f in range(8)],
                                reads=['ws%d' % sl, 'act2_%d' % b], writes=[pbk[py]])
                        if yi % 2:
                            S.op('act', ACTV(YB[b][:, st, dh * 512:(dh + 1) * 512], PB[py][:, :], AF.Copy), reads=[pbk[py]], writes=['yb%d' % b])
                        else:
                            S.op('dve', CP(YB[b][:, st, dh * 512:(dh + 1) * 512], PB[py][:, :]), reads=[pbk[py]], writes=['yb%d' % b])
                S.dma('sp', cyb[b], DMA(ys_d[e_ * CAP:(e_ + 1) * CAP, :].rearrange("(a p) n -> p a n", p=128), YB[b][:]),
                      reads=['yb%d' % b], writes=['ys%d' % e_])
                load_xe(e_ + 2)
            S.barrier()

        with ExitStack() as p6f:
            GATET = T(p6f, "gatet", [32, S_LEN], F32)
            B2R = T(p6f, "b2r", [32, D], F32)
            S.dma('sp', S.chan(), DMA(B2R[:], b2_d), writes=['b2r'])
            for g4 in range(4):
                S.group('pe', [TR(PB[4][0:32, q * 128:(q + 1) * 128], GATE[:, g4 * 4 + q, :], ident_f[:]) for q in range(4)],
                        reads=['gate', 'ident_f'], writes=[pbk[4]])
                S.op('act', ACTV(GATET[0:32, g4 * 512:(g4 + 1) * 512], PB[4][0:32, :], AF.Copy), reads=[pbk[4]], writes=['gatet'])
            YG = [[T(p6f, "yg%d_%d" % (k, i), [128, D], BF16) for i in range(2)] for k in range(4)]
            cyg = [[S.chan('yg%d_%d' % (k, i)) for i in range(2)] for k in range(4)]
            for k in range(4):
                for i in range(2):
                    S.op('pool', MS(YG[k][i][:], 0.0), writes=['yg%d_%d' % (k, i)])
            AC = [T(p6f, "ac%d" % i, [128, D], F32) for i in range(2)]
            X1 = [T(p6f, "x1b%d" % i, [128, D], F32) for i in range(2)]
            OT = [T(p6f, "ot%d" % i, [128, D], F32) for i in range(2)]
            JK2 = T(p6f, "jk2", [128, D], BF16)
            cl = [S.chan('x1l0'), S.chan('x1l1')]
            co = [S.chan('o0'), S.chan('o1')]
            for tt in range(NT):
                b = tt % 2
                S.dma('sp', cl[b], DMA(X1[b][:], x1_d[tt * 128:(tt + 1) * 128, :]), writes=['x1b%d' % b])
                for k in range(4):
                    S.dma('pool', cyg[k][b], lambda e, tt=tt, k=k, b=b: e.indirect_dma_start(
                        out=YG[k][b][:, :], out_offset=None, in_=ys_d[:, :],
                        in_offset=bass.IndirectOffsetOnAxis(ap=IDXS[:, tt, k:k + 1], axis=0),
                        bounds_check=breg(e), oob_is_err=False), writes=['yg%d_%d' % (k, b)], drain=True)
                for hf in range(2):
                    pbi = 2 * b + hf
                    S.group('pe', [MM(PB[pbi][:, :], GATET[0:32, tt * 128:(tt + 1) * 128], B2R[0:32, hf * 512:(hf + 1) * 512])],
                            reads=['gatet', 'b2r'], writes=[pbk[pbi]])
                    S.op('dve', STT(AC[b][:, hf * 512:(hf + 1) * 512], YG[0][b][:, hf * 512:(hf + 1) * 512], GK[:, tt, 0:1], PB[pbi][:, :],
                                    ALU.mult, ALU.add), reads=['yg0_%d' % b, pbk[pbi]], writes=['ac%d' % b])
                for k in range(1, 4):
                    S.op('dve', STT(AC[b][:], YG[k][b][:], GK[:, tt, k:k + 1], AC[b][:], ALU.mult, ALU.add),
                         reads=['yg%d_%d' % (k, b), 'ac%d' % b], writes=['ac%d' % b])
                if dbg and tt == 0:
                    dump("acc0", AC[0][:], [128, D], reads=['ac0'])
                S.op('act', ACTV(JK2[:], AC[b][:], AF.Square, accum_out=rt6[:, 16:17]), reads=['ac%d' % b], writes=['jk2', 'f6a'])
                S.op('act', ACTV(rt6[:, 17:18], rt6[:, 16:17], AF.Sqrt, scale=1.0 / D, bias=epsc[:, 0:1]), reads=['f6a', 'epsc'], writes=['f6b'])
                S.op('dve', lambda e: e.reciprocal(out=rt6[:, 18:19], in_=rt6[:, 17:18]), reads=['f6b'], writes=['f6c'])
                S.op('dve', STT(OT[b][:], AC[b][:], rt6[:, 18:19], G2[:], ALU.mult, ALU.mult), reads=['ac%d' % b, 'f6c', 'g1'], writes=['ot%d' % b])
                S.op('dve', TT(OT[b][:], OT[b][:], X1[b][:], ALU.add), reads=['ot%d' % b, 'x1b%d' % b], writes=['ot%d' % b])
                S.dma('sp', co[b], DMA(out_d[tt * 128:(tt + 1) * 128, :], OT[b][:]), reads=['ot%d' % b])
        S.finish()
        S.replay()
    return nc, dbg_out


_FRQ = None


def _host_inputs(inputs, b):
    f = lambda k: np.ascontiguousarray(np.asarray(inputs[k]))
    pos = np.asarray(inputs["positions"])[b].astype(np.int32)
    frq = np.zeros((128, 1), np.float32)
    j = np.arange(16, dtype=np.float64)
    fr = (10000.0 ** (-j / 16.0)) / (2 * np.pi)
    frq[64:80, 0] = fr
    frq[80:96, 0] = fr
    g4 = np.concatenate([f("g_pre_mix")[0], f("g_post_mix")[0], f("g_pre_ffn")[0], f("g_post_ffn")[0]])[None, :]
    return {
        "x": f("x")[b],
        "c": np.ascontiguousarray(f("c")[b].reshape(8, 128).T),
        "posr": np.ascontiguousarray(pos),
        "posc": np.ascontiguousarray(pos.reshape(NT, 128).T),
        "frq": frq,
        "w_ada": f("w_ada")[0], "b_ada": f("b_ada"),
        "g4": np.ascontiguousarray(g4),
        "w_in": f("w_in")[0],
        "gq": np.ascontiguousarray(f("g_q_a")[0].reshape(2, 128).T),
        "gkv": np.ascontiguousarray(f("g_kv_a")[0].reshape(1, 128).T),
        "w_q_b": f("w_q_b")[0], "w_kv_b": f("w_kv_b")[0], "w_o": f("w_o")[0],
        "w_router": f("w_router")[0], "b_router": f("b_router")[0],
        "w_mlp1": f("w_mlp1")[0], "b_mlp1": f("b_mlp1")[0],
        "w_mlp2": f("w_mlp2")[0], "b_mlp2": f("b_mlp2")[0],
    }


def kernel(**inputs):
    nc, _ = build_program(stage=99, dbg=False)
    in_maps = [_host_inputs(inputs, b) for b in range(8)]
    res = run_bass_kernel_spmd(nc, in_maps, core_ids=list(range(8)))
    out = np.stack([np.asarray(r["out"]) for r in res.results], axis=0)
    return out.astype(np.float32)
```

```python
import os
import math
from contextlib import ExitStack
import numpy as np
import concourse.bass as bass
import concourse.mybir as mybir
from concourse.bass_utils import run_bass_kernel_spmd

F32 = mybir.dt.float32
BF16 = mybir.dt.bfloat16
I32 = mybir.dt.int32
ALU = mybir.AluOpType
AF = mybir.ActivationFunctionType
AX = mybir.AxisListType

S_LEN = 2048
D = 1024
NT = 16
NC4 = 4
EPS = 1e-6
NEXP = 32


class Sched:
    ENG = ('pe', 'act', 'dve', 'pool', 'sp')

    def __init__(self, nc, es):
        self.nc = nc
        self.es = es
        self.sem = {}
        self.count = {}
        self.seen = {}
        self.stream = {e: [] for e in self.ENG}
        for e in self.ENG:
            self.sem[e] = es.enter_context(nc.semaphore('c_' + e))
            self.count[e] = 0
            self.seen[e] = {}
        self.last_w = {}
        self.readers = {}
        self.nchan = 0

    def chan(self, name=None):
        self.nchan += 1
        key = 'ch%d_%s' % (self.nchan, name or '')
        self.sem[key] = self.es.enter_context(self.nc.semaphore('d%d' % self.nchan))
        self.count[key] = 0
        return key

    def _deps(self, eng, reads, writes):
        deps = {}

        def add(e, i):
            if i > deps.get(e, 0):
                deps[e] = i
        for k in reads:
            lw = self.last_w.get(k)
            if lw is not None:
                add(*lw)
        for k in writes:
            lw = self.last_w.get(k)
            if lw is not None:
                add(*lw)
            for e, i in self.readers.get(k, {}).items():
                if e != eng:
                    add(e, i)
        return deps

    def _emit_waits(self, eng, deps):
        seen = self.seen[eng]
        for e, i in deps.items():
            if seen.get(e, 0) >= i:
                continue
            seen[e] = i
            self.stream[eng].append(('wait', self.sem[e], i))

    def _mark(self, who, idx, reads, writes):
        for k in reads:
            self.readers.setdefault(k, {})[who] = idx
        for k in writes:
            self.last_w[k] = (who, idx)
            self.readers[k] = {}

    def op(self, eng, fn, reads=(), writes=()):
        deps = self._deps(eng, reads, writes)
        self._emit_waits(eng, deps)
        self.count[eng] += 1
        self.stream[eng].append(('inst', fn, self.sem[eng], 1))
        self._mark(eng, self.count[eng], reads, writes)

    def group(self, eng, fns, reads=(), writes=()):
        deps = self._deps(eng, reads, writes)
        self._emit_waits(eng, deps)
        self.count[eng] += 1
        for f in fns[:-1]:
            self.stream[eng].append(('inst', f, None, 0))
        self.stream[eng].append(('inst', fns[-1], self.sem[eng], 1))
        self._mark(eng, self.count[eng], reads, writes)

    def dma(self, eng, ch, fn, reads=(), writes=(), drain=False):
        deps = self._deps(ch, reads, writes)
        if drain and self.count[ch] > 0:
            deps[ch] = self.count[ch]
        self._emit_waits(eng, deps)
        self.count[ch] += 16
        self.stream[eng].append(('inst', fn, self.sem[ch], 16))
        self._mark(ch, self.count[ch], reads, writes)

    def barrier(self):
        for eng in self.ENG:
            deps = {e: c for e, c in self.count.items() if c > 0 and e not in getattr(self, 'hold', ())}
            self._emit_waits(eng, deps)
        self.last_w = {}
        self.readers = {}

    def finish(self):
        for eng in self.ENG:
            deps = {e: c for e, c in self.count.items() if c > 0}
            self._emit_waits(eng, deps)

    def replay(self):
        nc = self.nc
        streams = self.stream

        def run(engobj, lst):
            for it in lst:
                if it[0] == 'wait':
                    engobj.wait_ge(it[1], it[2])
                else:
                    ins = it[1](engobj)
                    if it[2] is not None:
                        ins.then_inc(it[2], it[3])
        with nc.Block() as block:
            @block.tensor
            def _(e):
                run(e, streams['pe'])

            @block.scalar
            def _(e):
                run(e, streams['act'])

            @block.vector
            def _(e):
                run(e, streams['dve'])

            @block.gpsimd
            def _(e):
                run(e, streams['pool'])

            @block.sync
            def _(e):
                run(e, streams['sp'])


def MM(out, lhsT, rhs, start=True, stop=True):
    return lambda e: e.matmul(out, lhsT=lhsT, rhs=rhs, start=start, stop=stop)


def TR(out, in_, ident):
    return lambda e: e.transpose(out=out, in_=in_, identity=ident)


def ACTV(out, in_, func, **kw):
    return lambda e: e.activation(out=out, in_=in_, func=func, **kw)


def TS(out, in0, s1, s2, op0, op1=None, **kw):
    if op1 is None:
        return lambda e: e.tensor_scalar(out=out, in0=in0, scalar1=s1, scalar2=None, op0=op0, **kw)
    return lambda e: e.tensor_scalar(out=out, in0=in0, scalar1=s1, scalar2=s2, op0=op0, op1=op1, **kw)


def TT(out, in0, in1, op):
    return lambda e: e.tensor_tensor(out=out, in0=in0, in1=in1, op=op)


def STT(out, in0, scalar, in1, op0, op1, **kw):
    return lambda e: e.scalar_tensor_tensor(out=out, in0=in0, scalar=scalar, in1=in1, op0=op0, op1=op1, **kw)


def CP(out, in_):
    return lambda e: e.tensor_copy(out=out, in_=in_)


def MS(ap, v):
    return lambda e: e.memset(ap, v)


def DMA(out, in_):
    return lambda e: e.dma_start(out=out, in_=in_)


def build_program(stage=99, dbg=False):
    nc = bass.Bass("TRN2", target_bir_lowering=False)

    def din(name, shape, dt):
        return nc.dram_tensor(name, shape, dt, kind="ExternalInput").ap()
    x_d = din("x", [S_LEN, D], F32)
    c_d = din("c", [128, 8], F32)
    posr_d = din("posr", [S_LEN], I32)
    posc_d = din("posc", [128, NT], I32)
    frq_d = din("frq", [128, 1], F32)
    w_ada_d = din("w_ada", [D, 6 * D], F32)
    b_ada_d = din("b_ada", [1, 6 * D], F32)
    g4_d = din("g4", [1, 4 * D], F32)
    w_in_d = din("w_in", [D, 1480], F32)
    gq_d = din("gq", [128, 2], F32)
    gkv_d = din("gkv", [128, 1], F32)
    w_q_b_d = din("w_q_b", [256, 768], F32)
    w_kv_b_d = din("w_kv_b", [128, 1024], F32)
    w_o_d = din("w_o", [D, D], F32)
    w_r_d = din("w_router", [D, NEXP], F32)
    b_r_d = din("b_router", [NEXP], F32)
    w1_d = din("w_mlp1", [NEXP, D, 2 * D], F32)
    b1_d = din("b_mlp1", [NEXP, 2 * D], F32)
    w2_d = din("w_mlp2", [NEXP, D, D], F32)
    b2_d = din("b_mlp2", [NEXP, D], F32)
    out_d = nc.dram_tensor("out", [S_LEN, D], F32, kind="ExternalOutput").ap()
    x1_d = nc.dram_tensor("x1s", [S_LEN, D], F32, kind="Internal").ap()
    dbg_out = {}

    with ExitStack() as es:
        S = Sched(nc, es)

        AW = 53000
        arena_t = es.enter_context(nc.sbuf_tensor("arena", [128, AW], F32))
        ast = {'off': 0, 'peak': 0}

        def _rel(m):
            ast['off'] = m

        def T(stack, name, shape, dt):
            P_ = shape[0]
            n = 1
            for d_ in shape[1:]:
                n *= d_
            esz = 4 if dt in (F32, I32) else 2
            words = (n * esz + 3) // 4
            words = (words + 7) // 8 * 8
            off = ast['off']
            assert off + words <= AW, "SBUF arena overflow at %s: %d + %d" % (name, off, words)
            stack.callback(_rel, off)
            ast['off'] = off + words
            ast['peak'] = max(ast['peak'], ast['off'])
            ap = arena_t[0:P_, off:off + words]
            if dt != F32:
                ap = ap.bitcast(dt)
            ap = ap[:, 0:n]
            if len(shape) > 2:
                names = ['a%d' % i for i in range(len(shape) - 1)]
                pat = "p (%s) -> p %s" % (' '.join(names), ' '.join(names))
                ap = ap.rearrange(pat, **{nm: sz for nm, sz in zip(names[:-1], shape[1:-1])})
            return ap

        PB = [es.enter_context(nc.psum_tensor("pb%d" % i, [128, 512], F32)) for i in range(7)]
        PTB = es.enter_context(nc.psum_tensor("ptb", [128, 1024], BF16))
        pbk = ['pb%d' % i for i in range(7)]

        def dump(name, ap, shape, dt=F32, reads=()):
            if not dbg:
                return
            t = nc.dram_tensor("dbg_" + name, list(shape), dt, kind="ExternalOutput").ap()
            dbg_out[name] = t
            ch = S.chan('dbg')
            S.dma('sp', ch, DMA(t, ap), reads=list(reads))

        ident_f = T(es, "ident_f", [128, 128], F32)
        ident_b = T(es, "ident_b", [128, 128], BF16)
        ones_f = T(es, "ones_f", [128, 128], F32)
        tri_b = T(es, "tri_b", [128, 128], BF16)
        COLS = T(es, "cols", [128, 32], F32)
        G1 = T(es, "g1", [128, D], F32)
        G2 = T(es, "g2", [128, D], F32)
        HT = T(es, "ht", [128, 8, S_LEN], BF16)
        small = T(es, "small", [128, 64], F32)
        epsc = T(es, "epsc", [128, 1], F32)
        hpi = T(es, "hpi", [128, 1], F32)
        S.op('pool', MS(hpi[:], math.pi / 2), writes=['hpi'])
        ZT = T(es, "zt", [128, D], BF16)
        S.op('pool', MS(ZT[:], 0.0), writes=['zt'])

        S.op('pool', MS(ident_f[:], 1.0), writes=['ident_f'])
        S.op('pool', lambda e: e.affine_select(out=ident_f[:], in_=ident_f[:], pattern=[[-1, 128]],
                                               compare_op=ALU.is_equal, fill=0.0, base=0, channel_multiplier=1),
             reads=['ident_f'], writes=['ident_f'])
        S.op('dve', CP(ident_b[:], ident_f[:]), reads=['ident_f'], writes=['ident_b'])
        S.op('pool', MS(ones_f[:], 1.0), writes=['ones_f'])
        S.op('pool', MS(epsc[:], EPS), writes=['epsc'])
        S.op('pool', MS(tri_b[:], 1.0), writes=['tri_b'])
        S.op('pool', lambda e: e.affine_select(out=tri_b[:], in_=tri_b[:], pattern=[[1, 128]],
                                               compare_op=ALU.is_ge, fill=0.0, base=0, channel_multiplier=-1),
             reads=['tri_b'], writes=['tri_b'])


        with ExitStack() as p0:
            CT = T(p0, "ct", [128, 8], F32)
            CS = T(p0, "cs", [128, 8], F32)
            MODR = T(p0, "modr", [1, 6 * D], F32)
            BADA = T(p0, "bada", [1, 6 * D], F32)
            GROW = T(p0, "grow", [1, 4 * D], F32)
            ROWS = T(p0, "rows", [1, 4 * D], F32)
            NWB = 4
            WAD = [T(p0, "wad%d" % i, [128, 8, 512], BF16) for i in range(NWB)]
            cw = [S.chan('wad%d' % i) for i in range(NWB)]
            CSB = T(p0, "csb", [128, 8], BF16)
            S.dma('sp', S.chan(), DMA(CT[:], c_d), writes=['ct'])
            S.dma('sp', S.chan(), DMA(BADA[:], b_ada_d), writes=['bada'])
            S.dma('sp', S.chan(), DMA(GROW[:], g4_d), writes=['grow'])
            S.op('act', ACTV(CS[:], CT[:], AF.Silu), reads=['ct'], writes=['cs'])
            S.op('dve', CP(CSB[:], CS[:]), reads=['cs'], writes=['csb'])
            wv = w_ada_d.rearrange("(k p) n -> p k n", p=128)
            for n in range(12):
                b = n % NWB
                pb_ = n % 2
                S.dma('pool', cw[b], DMA(WAD[b][:], wv[:, :, n * 512:(n + 1) * 512]), writes=['wad%d' % b])
                S.group('pe', [MM(PB[pb_][0:1, :], CSB[:, k:k + 1], WAD[b][:, k, :], k == 0, k == 7) for k in range(8)],
                        reads=['csb', 'wad%d' % b], writes=[pbk[pb_]])
                S.op('dve', TT(MODR[0:1, n * 512:(n + 1) * 512], PB[pb_][0:1, :], BADA[0:1, n * 512:(n + 1) * 512], ALU.add),
                     reads=[pbk[pb_], 'bada'], writes=['modr%d' % n])
            mk = lambda a, b_: ['modr%d' % n for n in range(a // 512, (b_ + 511) // 512)]
            sh1, sc1, gt1, sh2, sc2, gt2 = [(i * D, (i + 1) * D) for i in range(6)]
            S.op('dve', STT(ROWS[0:1, 0:D], MODR[0:1, sc1[0]:sc1[1]], 1.0, GROW[0:1, 0:D], ALU.add, ALU.mult),
                 reads=mk(*sc1) + ['grow'], writes=['rows0'])
            S.op('dve', TT(ROWS[0:1, D:2 * D], MODR[0:1, gt1[0]:gt1[1]], GROW[0:1, D:2 * D], ALU.mult),
                 reads=mk(*gt1) + ['grow'], writes=['rows1'])
            S.op('dve', STT(ROWS[0:1, 2 * D:3 * D], MODR[0:1, sc2[0]:sc2[1]], 1.0, GROW[0:1, 2 * D:3 * D], ALU.add, ALU.mult),
                 reads=mk(*sc2) + ['grow'], writes=['rows2'])
            S.op('dve', TT(ROWS[0:1, 3 * D:4 * D], MODR[0:1, gt2[0]:gt2[1]], GROW[0:1, 3 * D:4 * D], ALU.mult),
                 reads=mk(*gt2) + ['grow'], writes=['rows3'])
            srcs = [(ROWS, 0), (MODR, sh1[0]), (ROWS, 2 * D), (MODR, sh2[0])]
            fns = []
            for v, (tt_, off) in enumerate(srcs):
                for k in range(8):
                    fns.append(MM(PB[2][:, v * 8 + k:v * 8 + k + 1], tt_[0:1, off + k * 128:off + (k + 1) * 128],
                                  ones_f[0:1, 0:1]))
            S.group('pe', fns, reads=['rows0', 'rows2', 'ones_f'] + mk(*sh1) + mk(*sh2), writes=[pbk[2]])
            S.op('dve', CP(COLS[:], PB[2][:, 0:32]), reads=[pbk[2]], writes=['cols'])
            for gi, (G, off) in enumerate([(G1, D), (G2, 3 * D)]):
                for hf in range(2):
                    pbi = 3 + hf
                    S.group('pe', [MM(PB[pbi][:, :], ones_f[0:1, 0:128], ROWS[0:1, off + hf * 512:off + (hf + 1) * 512])],
                            reads=['rows1', 'rows3', 'ones_f'], writes=[pbk[pbi]])
                    S.op('act', ACTV(G[:, hf * 512:(hf + 1) * 512], PB[pbi][:, :], AF.Copy), reads=[pbk[pbi]],
                         writes=['g%d' % gi])
            dump("modr", MODR[:], [1, 6 * D], reads=mk(0, 6 * D))
            dump("cols", COLS[:], [128, 32], reads=['cols'])
            dump("g1", G1[:], [128, D], reads=['g0'])
            S.barrier()
        A1, B1c, A2, B2c = COLS[:, 0:8], COLS[:, 8:16], COLS[:, 16:24], COLS[:, 24:32]

        def norm_transpose(stk, load_tile, A, Bc, tag, dst, pre=None, post=None):
            XB = [T(stk, "%s_xb%d" % (tag, i), [128, D], F32) for i in range(4)]
            XN = [T(stk, "%s_xn%d" % (tag, i), [128, D], BF16) for i in range(4)]
            ST = T(stk, "%s_st" % tag, [128, 16], F32)
            for tc in range(NC4):
                for q in range(4):
                    tt = tc * 4 + q
                    load_tile(tt, XB[q], '%s_xb%d' % (tag, q))
                    S.op('act', ACTV(XN[q][:], XB[q][:], AF.Square, accum_out=ST[:, q:q + 1]),
                         reads=['%s_xb%d' % (tag, q)], writes=['%s_xn%d' % (tag, q), '%s_ss%d' % (tag, q)])
                S.op('act', ACTV(ST[:, 4:8], ST[:, 0:4], AF.Sqrt, scale=1.0 / D, bias=epsc[:, 0:1]),
                     reads=['%s_ss%d' % (tag, q) for q in range(4)] + ['epsc'], writes=['%s_sd' % tag])
                S.op('dve', lambda e: e.reciprocal(out=ST[:, 8:12], in_=ST[:, 4:8]), reads=['%s_sd' % tag],
                     writes=['%s_rs' % tag])
                for q in range(4):
                    S.op('dve', TS(XN[q][:], XB[q][:], ST[:, 8 + q:9 + q], None, ALU.mult),
                         reads=['%s_xb%d' % (tag, q), '%s_rs' % tag], writes=['%s_xn%d' % (tag, q)])
                    if pre is not None:
                        pre(tc * 4 + q, XN[q], '%s_xn%d' % (tag, q))
                for kp in range(4):
                    fns = []
                    for kk in range(2):
                        k = kp * 2 + kk
                        for q in range(4):
                            fns.append(TR(PTB[:, kk * 512 + q * 128:kk * 512 + (q + 1) * 128],
                                          XN[q][:, k * 128:(k + 1) * 128], ident_b[:]))
                    S.group('pe', fns, reads=['%s_xn%d' % (tag, q) for q in range(4)] + ['ident_b'], writes=['ptb'])
                    for kk in range(2):
                        k = kp * 2 + kk
                        if kk == 0:
                            S.op('act', ACTV(dst[:, k, tc * 512:(tc + 1) * 512], PTB[:, 0:512], AF.Identity,
                                             scale=A[:, k:k + 1], bias=Bc[:, k:k + 1]),
                                 reads=['ptb', 'cols'], writes=['%s_d%d' % (tag, tc)])
                        else:
                            S.op('dve', TS(dst[:, k, tc * 512:(tc + 1) * 512], PTB[:, 512:1024], A[:, k:k + 1],
                                           Bc[:, k:k + 1], ALU.mult, ALU.add),
                                 reads=['ptb', 'cols'], writes=['%s_d%d' % (tag, tc)])
                if post is not None:
                    post(tc)

        CAP = 512
        NSL = NEXP * CAP
        xs_d = nc.dram_tensor("xslots", [NSL, D], BF16, kind="Internal").ap()
        ys_d = nc.dram_tensor("yslots", [NSL, D], BF16, kind="Internal").ap()
        HTK = lambda tc: ['h_d%d' % tc]
        GATE = T(es, "gate", [128, NT, NEXP], F32)
        IDXS = T(es, "idxs", [128, NT, 4], I32)
        GK = T(es, "gk", [128, NT, 4], F32)
        rt6 = T(es, "rt6", [128, 64], F32)
        pmx = ExitStack()
        MIX = T(pmx, "mix", [128, 8, S_LEN], BF16)
        with ExitStack() as pa:
            cwt = S.chan('attw')
            WBt = T(pa, "wb", [128, 8, 776], BF16)
            FRQ = T(pa, "frq", [128, 1], F32)
            POSF = T(pa, "posf", [128, S_LEN], F32)
            PCI = T(pa, "pci", [128, NT], I32)
            NPC = T(pa, "npc", [128, NT], F32)
            KD = T(pa, "kd", [128, S_LEN], BF16)
            VD = T(pa, "vd", [128, NT, 2, 128], BF16)
            KI = T(pa, "ki", [96, S_LEN], BF16)
            GQ = T(pa, "gq", [128, 2], F32)
            GKV = T(pa, "gkv", [128, 1], F32)
            pm = ExitStack()
            WA = T(pm, "wa", [128, 8, 704], BF16)
            wiv = w_in_d.rearrange("(k p) n -> p k n", p=128)
            for (dst_, d0, s0, n_) in [(WA, 0, 0, 416), (WA, 416, 928, 256), (WA, 672, 1440, 32),
                                        (WBt, 512, 1184, 256), (WBt, 768, 1472, 8)]:
                S.dma('pool', cwt, DMA(dst_[:, :, d0:d0 + n_], wiv[:, :, s0:s0 + n_]), writes=['wa', 'wb'])
            wbq = WBt[:, :, 0:512].rearrange("p k (g kv d) -> p k g kv d", g=4, kv=2)
            for kv in range(2):
                for g in range(4):
                    c0_ = 416 + kv * 256 + g * 64
                    S.dma('pool', cwt, DMA(wbq[:, :, g, kv, :], wiv[:, :, c0_:c0_ + 64]), writes=['wa', 'wb'])
            WKI3 = T(pm, "wki3", [128, 8, 96], BF16)
            for r3 in range(3):
                S.op('pool', CP(WKI3[:, :, r3 * 32:(r3 + 1) * 32], WA[:, :, 672:704]), reads=['wa'], writes=['wki3'])
            WKR = T(pm, "wkr", [128, 8, 96], BF16)
            WKRS = T(pm, "wkrs", [128, 8, 96], BF16)
            S.op('pool', MS(WKR[:], 0.0), writes=['wkr'])
            S.op('pool', MS(WKRS[:], 0.0), writes=['wkrs'])
            S.op('pool', CP(WKR[:, :, 64:96], WA[:, :, 384:416]), reads=['wa'], writes=['wkr'])
            S.op('pool', TS(WKRS[:, :, 64:80], WA[:, :, 400:416], -1.0, None, ALU.mult), reads=['wa'], writes=['wkrs'])
            S.op('pool', CP(WKRS[:, :, 80:96], WA[:, :, 384:400]), reads=['wa'], writes=['wkrs'])
            WQB = T(pm, "wqb", [128, 2, 768], BF16)
            WQBS = T(pm, "wqbs", [128, 2, 768], BF16)
            WKVB = T(pm, "wkvb", [128, 1024], BF16)
            S.dma('pool', S.chan(), DMA(WQB[:], w_q_b_d.rearrange("(k p) n -> p k n", p=128)), writes=['wqb'])
            S.dma('pool', S.chan(), DMA(WKVB[:], w_kv_b_d), writes=['wkvb'])
            S.op('pool', MS(WQBS[:], 0.0), writes=['wqbs'])
            wq4 = WQB[:].rearrange("p k (h f) -> p k h f", h=8)
            wqs4 = WQBS[:].rearrange("p k (h f) -> p k h f", h=8)
            for m in range(2):
                S.op('pool', TS(wqs4[:, m, :, 64:80], wq4[:, m, :, 80:96], -1.0, None, ALU.mult), reads=['wqb'], writes=['wqbs'])
                S.op('pool', CP(wqs4[:, m, :, 80:96], wq4[:, m, :, 64:80]), reads=['wqb'], writes=['wqbs'])
            S.dma('sp', S.chan(), DMA(GQ[:], gq_d), writes=['gq'])
            S.dma('sp', S.chan(), DMA(GKV[:], gkv_d), writes=['gkv'])
            S.dma('sp', S.chan(), DMA(FRQ[:], frq_d), writes=['frq'])
            S.dma('sp', S.chan(), DMA(PCI[:], posc_d), writes=['pci'])
            S.op('dve', CP(NPC[:], PCI[:]), reads=['pci'], writes=['npc'])
            S.op('dve', TS(NPC[:], NPC[:], -1.0, None, ALU.mult), reads=['npc'], writes=['npc'])
            COS = T(pm, "cos", [128, S_LEN], F32)
            SIN = T(pm, "sin", [128, S_LEN], F32)
            pr = ExitStack()
            if True:
                POSI = T(pr, "posi", [128, S_LEN], I32)
                U = T(pr, "ru", [128, S_LEN], F32)
                UI = POSI
                U2 = T(pr, "ru2", [128, S_LEN], F32)
                S.dma('sp', S.chan(), DMA(POSI[:], posr_d.partition_broadcast(128)), writes=['posi'])
                S.op('dve', CP(POSF[:], POSI[:]), reads=['posi'], writes=['posf'])
                rs = slice(64, 96)
                S.op('dve', TS(U[rs, :], POSF[rs, :], FRQ[rs, 0:1], None, ALU.mult), reads=['posf', 'frq'], writes=['ru'])
                S.op('dve', CP(UI[rs, :], U[rs, :]), reads=['ru', 'posi'], writes=['posi'])
                S.op('dve', CP(U2[rs, :], UI[rs, :]), reads=['posi'], writes=['ru2'])
                S.op('dve', TT(U[rs, :], U[rs, :], U2[rs, :], ALU.subtract), reads=['ru', 'ru2'], writes=['ru'])
                S.op('dve', STT(U2[rs, :], U[rs, :], 0.5, U[rs, :], ALU.is_ge, ALU.subtract), reads=['ru'], writes=['ru2'])
                S.op('dve', STT(U[rs, :], U[rs, :], -0.5, U2[rs, :], ALU.is_lt, ALU.subtract), reads=['ru', 'ru2'], writes=['ru'])
                S.op('act', ACTV(SIN[rs, :], U[rs, :], AF.Sin, scale=2 * math.pi), reads=['ru'], writes=['sin'])
                S.op('act', ACTV(U2[rs, :], U[rs, :], AF.Abs, scale=2 * math.pi), reads=['ru'], writes=['ru2'])
                S.op('act', ACTV(COS[rs, :], U2[rs, :], AF.Sin, scale=-1.0, bias=hpi[rs, 0:1]), reads=['ru2', 'hpi'], writes=['cos'])
                dump("cos", COS[64:96, :], [32, S_LEN], reads=['cos'])
                dump("sin", SIN[64:96, :], [32, S_LEN], reads=['sin'])
            p1 = ExitStack()
            cx = [S.chan('x%d' % i) for i in range(4)]

            def load_x(tt, buf, key):
                S.dma('sp', cx[tt % 4], DMA(buf[:], x_d[tt * 128:(tt + 1) * 128, :]), writes=[key])
            norm_transpose(p1, load_x, A1, B1c, 'h', HT)
            dump("ht", HT[:], [128, 8, S_LEN], BF16, reads=['h_d%d' % i for i in range(4)])
            S.barrier()
            p1.close()
            pr.close()

            CQN = T(pm, "cqn", [128, 2, S_LEN], BF16)
            CKVN = T(pm, "ckvn", [128, S_LEN], BF16)
            KR = T(pm, "kr", [128, S_LEN], BF16)
            S.op('pool', MS(VD[:, :, :, 64:128], 1.0), writes=['vd'])

            with ExitStack() as pA:
                SQ = [T(pA, "sq%d" % i, [128, 512], F32) for i in range(3)]
                RQ = T(pA, "rq", [128, 512], F32)
                RKV = T(pA, "rkv", [128, 512], F32)
                TA = T(pA, "ta", [128, 512], F32)
                TB2 = T(pA, "tb2", [128, 512], F32)
                for tc in range(NC4):
                    ts_ = slice(tc * 512, (tc + 1) * 512)
                    hk = HTK(tc)
                    for m in range(3):
                        S.group('pe', [MM(PB[m][:, :], WA[:, k, m * 128:(m + 1) * 128], HT[:, k, ts_], k == 0, k == 7)
                                       for k in range(8)], reads=hk + ['wa'], writes=[pbk[m]])
                        S.op('act', ACTV(SQ[m][:], PB[m][:, :], AF.Square), reads=[pbk[m]], writes=['sq%d' % m])
                    S.group('pe', [MM(PB[3][:, :], ones_f[:, :], SQ[0][:], True, False),
                                   MM(PB[3][:, :], ones_f[:, :], SQ[1][:], False, True)],
                            reads=['sq0', 'sq1', 'ones_f'], writes=[pbk[3]])
                    S.group('pe', [MM(PB[4][:, :], ones_f[:, :], SQ[2][:], True, True)],
                            reads=['sq2', 'ones_f'], writes=[pbk[4]])
                    S.op('act', ACTV(RQ[:], PB[3][:, :], AF.Sqrt, scale=1.0 / 256, bias=epsc[:, 0:1]), reads=[pbk[3], 'epsc'], writes=['rq'])
                    S.op('act', ACTV(RKV[:], PB[4][:, :], AF.Sqrt, scale=1.0 / 128, bias=epsc[:, 0:1]), reads=[pbk[4], 'epsc'], writes=['rkv'])
                    S.op('dve', lambda e: e.reciprocal(out=RQ[:], in_=RQ[:]), reads=['rq'], writes=['rq'])
                    S.op('dve', lambda e: e.reciprocal(out=RKV[:], in_=RKV[:]), reads=['rkv'], writes=['rkv'])
                    for m in range(2):
                        S.op('dve', STT(CQN[:, m, ts_], PB[m][:, :], GQ[:, m:m + 1], RQ[:], ALU.mult, ALU.mult),
                             reads=[pbk[m], 'gq', 'rq'], writes=['cqn%d' % tc])
                    S.op('dve', STT(CKVN[:, ts_], PB[2][:, :], GKV[:, 0:1], RKV[:], ALU.mult, ALU.mult),
                         reads=[pbk[2], 'gkv', 'rkv'], writes=['ckvn%d' % tc])
                    S.group('pe', [MM(PB[5][0:96, :], WKR[:, k, :], HT[:, k, ts_], k == 0, k == 7) for k in range(8)],
                            reads=hk + ['wkr'], writes=[pbk[5]])
                    S.group('pe', [MM(PB[6][0:96, :], WKRS[:, k, :], HT[:, k, ts_], k == 0, k == 7) for k in range(8)],
                            reads=hk + ['wkrs'], writes=[pbk[6]])
                    S.op('dve', TT(TA[64:96, :], PB[5][64:96, :], COS[64:96, ts_], ALU.mult), reads=[pbk[5], 'cos'], writes=['ta'])
                    S.op('dve', TT(TB2[64:96, :], PB[6][64:96, :], SIN[64:96, ts_], ALU.mult), reads=[pbk[6], 'sin'], writes=['tb2'])
                    S.op('pool', TT(KR[64:96, ts_], TA[64:96, :], TB2[64:96, :], ALU.add), reads=['ta', 'tb2'], writes=['kr%d' % tc])
                    S.group('pe', [MM(PB[0][:, :], WA[:, k, 416:544], HT[:, k, ts_], k == 0, k == 7)
                                   for k in range(8)], reads=hk + ['wa'], writes=[pbk[0]])
                    S.op('act', ACTV(KD[:, ts_], PB[0][:, :], AF.Copy), reads=[pbk[0]], writes=['kd%d' % tc])
                    S.group('pe', [MM(PB[2][0:96, :], WKI3[:, k, :], HT[:, k, ts_], k == 0, k == 7) for k in range(8)],
                            reads=hk + ['wki3'], writes=[pbk[2]])
                    S.op('act', ACTV(KI[0:96, ts_], PB[2][0:96, :], AF.Copy), reads=[pbk[2]], writes=['ki%d' % tc])
                    fns = []
                    for q in range(4):
                        tt = tc * 4 + q
                        for k in range(8):
                            fns.append(MM(PB[3][:, q * 128:(q + 1) * 128], HT[:, k, tt * 128:(tt + 1) * 128], WA[:, k, 544:672],
                                          k == 0, k == 7))
                    S.group('pe', fns, reads=hk + ['wa'], writes=[pbk[3]])
                    S.op('act', ACTV(VD[:, tc * 4:(tc + 1) * 4, :, 0:64],
                                     PB[3][:, :].rearrange("p (q v d) -> p q v d", q=4, v=2), AF.Copy),
                         reads=[pbk[3]], writes=['vd'])
                dump("cqn", CQN[:], [128, 2, S_LEN], BF16, reads=['cqn%d' % i for i in range(4)])
                dump("ckvn", CKVN[:], [128, S_LEN], BF16, reads=['ckvn%d' % i for i in range(4)])
                dump("kr", KR[64:96, :], [32, S_LEN], BF16, reads=['kr%d' % i for i in range(4)])
                dump("vd", VD[:], [128, NT, 2, 128], BF16, reads=['vd'])
                S.barrier()
            if stage <= 2:
                S.finish(); S.replay()
                return nc, dbg_out

            with ExitStack() as pB:
                QT = [T(pB, "qt0", [96, S_LEN], BF16)] * 2
                KT = [T(pB, "kt0", [96, S_LEN], BF16)] * 2
                VA = [T(pB, "va0", [128, NT, 128], BF16)] * 2
                TA = T(pB, "mta", [128, 512], F32)
                TB2 = T(pB, "mtb", [128, 512], F32)
                PTl = [T(pB, "pt%d" % i, [128, 512], BF16) for i in range(3)]
                RT = [T(pB, "rt%d" % i, [64, 512], F32) for i in range(2)]
                S.op('pool', MS(VA[0][:, :, 64:128], 1.0), writes=['va0'])
                scale = (64 + 32) ** -0.5
                cz = S.chan('zero')
                S.hold = {cz}
                for a_ in range(NSL // 128):
                    S.dma('sp', cz, DMA(xs_d[a_ * 128:(a_ + 1) * 128, :], ZT[:]), reads=['zt'], writes=['xs_z%d' % a_])
                pti = 0
                for h in range(8):
                    hb = 0
                    qk, kk_, vk = 'qt%d' % hb, 'kt%d' % hb, 'va%d' % hb
                    for tc in range(NC4):
                        ts_ = slice(tc * 512, (tc + 1) * 512)
                        S.group('pe', [MM(PB[0][0:96, :], WQB[:, m, h * 96:(h + 1) * 96], CQN[:, m, ts_], m == 0, m == 1) for m in range(2)],
                                reads=['wqb', 'cqn%d' % tc], writes=[pbk[0]])
                        S.group('pe', [MM(PB[1][0:96, :], WQBS[:, m, h * 96:(h + 1) * 96], CQN[:, m, ts_], m == 0, m == 1) for m in range(2)],
                                reads=['wqbs', 'cqn%d' % tc], writes=[pbk[1]])
                        S.op('act', ACTV(QT[hb][0:64, ts_], PB[0][0:64, :], AF.Copy), reads=[pbk[0]], writes=[qk])
                        S.op('dve', TT(TA[64:96, :], PB[0][64:96, :], COS[64:96, ts_], ALU.mult), reads=[pbk[0], 'cos'], writes=['mta'])
                        S.op('dve', TT(TB2[64:96, :], PB[1][64:96, :], SIN[64:96, ts_], ALU.mult), reads=[pbk[1], 'sin'], writes=['mtb'])
                        S.op('pool', TT(QT[hb][64:96, ts_], TA[64:96, :], TB2[64:96, :], ALU.add), reads=['mta', 'mtb'], writes=[qk])
                        S.group('pe', [MM(PB[2][0:64, :], WKVB[:, h * 128:h * 128 + 64], CKVN[:, ts_])],
                                reads=['wkvb', 'ckvn%d' % tc], writes=[pbk[2]])
                        S.op('act', ACTV(KT[hb][0:64, ts_], PB[2][0:64, :], AF.Copy), reads=[pbk[2]], writes=[kk_])
                        S.op('pool', CP(KT[hb][64:96, ts_], KR[64:96, ts_]), reads=['kr%d' % tc], writes=[kk_])
                        S.group('pe', [MM(PB[2][:, q * 64:(q + 1) * 64], CKVN[:, (tc * 4 + q) * 128:(tc * 4 + q + 1) * 128],
                                          WKVB[:, h * 128 + 64:(h + 1) * 128]) for q in range(4)],
                                reads=['wkvb', 'ckvn%d' % tc], writes=[pbk[2]])
                        S.op('dve', CP(VA[hb][:, tc * 4:(tc + 1) * 4, 0:64], PB[2][:, 0:256].rearrange("p (q d) -> p q d", q=4)),
                             reads=[pbk[2]], writes=[vk])
                    if dbg and h == 0:
                        dump("qt0", QT[0][:], [96, S_LEN], BF16, reads=[qk])
                        dump("kt0", KT[0][:], [96, S_LEN], BF16, reads=[kk_])
                        dump("va0", VA[0][:], [128, NT, 128], BF16, reads=[vk])
                    blocks = [(c, j) for c in range(NC4) for j in range(4 * c + 4)]
                    LA = 3

                    def emit_score(bi, h=h, qk=qk, kk_=kk_):
                        c, j = blocks[bi]
                        c0 = max(0, j - 4 * c) * 128
                        ps = 2 + (bi % 3)
                        S.group('pe', [MM(PB[ps][:, c0:512], KT[0][0:96, j * 128:(j + 1) * 128],
                                          QT[0][0:96, c * 512 + c0:(c + 1) * 512])],
                                reads=[qk, kk_], writes=[pbk[ps]])
                    for bi in range(min(LA, len(blocks))):
                        emit_score(bi)
                    for bi, (c, j) in enumerate(blocks):
                        po = 5 + (c % 2)
                        nj = 4 * c + 4
                        r = j - 4 * c
                        c0 = max(0, r) * 128
                        ps = 2 + (bi % 3)
                        pt = PTl[bi % 3]
                        ptk = 'pt%d' % (bi % 3)
                        S.op('act', ACTV(pt[:, c0:512], PB[ps][:, c0:512], AF.Exp, scale=scale), reads=[pbk[ps]], writes=[ptk])
                        if r >= 0:
                            S.op('pool', TT(pt[:, c0:c0 + 128], pt[:, c0:c0 + 128], tri_b[:], ALU.mult),
                                 reads=[ptk, 'tri_b'], writes=[ptk])
                        S.group('pe', [MM(PB[po][:, c0:512], VA[0][:, j, :], pt[:, c0:512], j == 0, j == nj - 1)],
                                reads=[vk, ptk], writes=[pbk[po]])
                        if bi + LA < len(blocks):
                            emit_score(bi + LA)
                        if j == nj - 1:
                            rt = RT[c % 2]
                            rk = 'rt%d' % (c % 2)
                            S.op('dve', lambda e, rt=rt, po=po: e.reciprocal(out=rt[0:64, :], in_=PB[po][64:128, :]),
                                 reads=[pbk[po]], writes=[rk])
                            S.op('dve', TT(MIX[(h % 2) * 64:(h % 2) * 64 + 64, h // 2, c * 512:(c + 1) * 512],
                                           PB[po][0:64, :], rt[0:64, :], ALU.mult),
                                 reads=[pbk[po], rk], writes=['mix%d' % c])
                dump("mix_mla", MIX[:, 0:4, :], [128, 4, S_LEN], BF16, reads=['mix%d' % c for c in range(4)])
                S.barrier()
            pm.close()
            if stage <= 3:
                S.finish(); S.replay()
                return nc, dbg_out

            with ExitStack() as pC:
                QD = T(pC, "qd", [128, 4, 512], BF16)
                QI = T(pC, "qi", [96, 3, 512], BF16)
                WI = T(pC, "wi", [128, 4, 8], F32)
                IDX2 = [T(pC, "idx%d" % i, [128, S_LEN], F32) for i in range(2)]
                MID = T(pC, "mid", [128, 2], F32)
                CNT = T(pC, "cnt", [128, 2], F32)
                TQ = T(pC, "tq", [128, 2], F32)
                RL = [T(pC, "rl%d" % i, [128, 512], F32) for i in range(2)]
                M01 = [T(pC, "m01_%d" % i, [128, S_LEN], BF16) for i in range(2)]
                MT2 = [T(pC, "mt%d" % i, [128, NT, 512], BF16) for i in range(2)]
                THR = T(pC, "thr", [128, 2], F32)
                DT = [T(pC, "dt%d" % i, [128, 512], F32) for i in range(3)]
                EE = [T(pC, "ee%d" % i, [128, 512], BF16) for i in range(3)]
                PM = [T(pC, "pm%d" % i, [128, 512], BF16) for i in range(3)]
                RT = [T(pC, "drt%d" % i, [64, 512], F32) for i in range(2)]
                SLD = T(pC, "sld", [128, 8, 128], F32)
                for hd in range(8):
                    S.op('pool', TS(SLD[:, hd, :], ident_f[:], -8.0 * 2.0 ** (-(hd + 1)), None, ALU.mult),
                         reads=['ident_f'], writes=['sld'])
                rlc = [0]

                def stage1_proj(c):
                    ts_ = slice(c * 512, (c + 1) * 512)
                    hk = HTK(c)
                    for slot in range(3):
                        nh = 3 if slot < 2 else 2
                        S.group('pe', [MM(PB[2][0:nh * 32, :], WBt[:, k, 512 + slot * 96:512 + slot * 96 + nh * 32], HT[:, k, ts_], k == 0, k == 7)
                                       for k in range(8)], reads=hk + ['wb'], writes=[pbk[2]])
                        S.op('dve', TS(QI[0:nh * 32, slot, :], PB[2][0:nh * 32, :], 32 ** -0.5, None, ALU.mult), reads=[pbk[2]], writes=['qi'])
                    fns = []
                    for q in range(4):
                        tt = c * 4 + q
                        for k in range(8):
                            fns.append(MM(PB[2][:, q * 8:(q + 1) * 8], HT[:, k, tt * 128:(tt + 1) * 128], WBt[:, k, 768:776], k == 0, k == 7))
                    S.group('pe', fns, reads=hk + ['wb'], writes=[pbk[2]])
                    S.op('dve', TS(WI[:].rearrange("p q h -> p (q h)"), PB[2][:, 0:32], 8 ** -0.5, None, ALU.mult),
                         reads=[pbk[2]], writes=['wi'])

                def stage1_idx(c, q0):
                    for t_ in range(2):
                        q = q0 + t_
                        i = c * 4 + q
                        ncols = (i + 1) * 128
                        nsc = (ncols + 511) // 512
                        IDX = IDX2[t_]
                        ik = 'idx%d' % t_
                        for hd in range(8):
                            grp, slot = hd % 3, hd // 3
                            for sc in range(nsc):
                                w = min(512, ncols - sc * 512)
                                pi = rlc[0] % 2
                                rl = RL[pi]
                                rlk = 'rl%d' % pi
                                rlc[0] += 1
                                S.group('pe', [MM(PB[pi][:, 0:w], QI[grp * 32:(grp + 1) * 32, slot, q * 128:(q + 1) * 128],
                                                  KI[grp * 32:(grp + 1) * 32, sc * 512:sc * 512 + w])],
                                        reads=['qi'] + ['ki%d' % sc], writes=[pbk[pi]])
                                S.op('act', ACTV(rl[:, 0:w], PB[pi][:, 0:w], AF.Relu), reads=[pbk[pi]], writes=[rlk])
                                if hd == 0:
                                    S.op('dve', TS(IDX[:, sc * 512:sc * 512 + w], rl[:, 0:w], WI[:, q, 0:1], None, ALU.mult),
                                         reads=[rlk, 'wi'], writes=[ik])
                                else:
                                    S.op('dve', STT(IDX[:, sc * 512:sc * 512 + w], rl[:, 0:w], WI[:, q, hd:hd + 1],
                                                    IDX[:, sc * 512:sc * 512 + w], ALU.mult, ALU.add),
                                         reads=[rlk, 'wi', ik], writes=[ik])
                        S.op('pool', lambda e, i=i, IDX=IDX: e.affine_select(out=IDX[:, i * 128:(i + 1) * 128], in_=IDX[:, i * 128:(i + 1) * 128],
                                                                 pattern=[[-1, 128]], compare_op=ALU.is_ge, fill=-1e30,
                                                                 base=0, channel_multiplier=1),
                             reads=[ik], writes=[ik])

                def stage1_bisect(c, q0):
                    i0 = c * 4 + q0
                    if i0 >= 2:
                        S.op('dve', MS(MID[:, 0:2], 0.0), writes=['mid'])
                        NIT = 24
                        for it in range(NIT):
                            w_it = 64.0 / (2.0 ** it)
                            for t_ in range(2):
                                ncols = (i0 + t_ + 1) * 128
                                S.op('dve', lambda e, t_=t_, ncols=ncols: e.tensor_scalar(
                                    out=M01[t_][:, 0:ncols], in0=IDX2[t_][:, 0:ncols], scalar1=MID[:, t_:t_ + 1], scalar2=0.0,
                                    op0=ALU.is_ge, op1=ALU.add, accum_out=CNT[:, t_:t_ + 1]),
                                    reads=['idx%d' % t_, 'mid'], writes=['m01_%d' % t_, 'cnt%d' % t_])
                            S.op('dve', TS(TQ[:, 0:2], CNT[:, 0:2], 256.0, w_it, ALU.is_ge, ALU.mult), reads=['cnt0', 'cnt1'], writes=['tq'])
                            if it < NIT - 1:
                                S.op('dve', STT(MID[:, 0:2], TQ[:, 0:2], -w_it / 2, MID[:, 0:2], ALU.add, ALU.add), reads=['tq', 'mid'], writes=['mid'])
                            else:
                                S.op('dve', STT(THR[:, 0:2], TQ[:, 0:2], -w_it, MID[:, 0:2], ALU.add, ALU.add), reads=['tq', 'mid'], writes=['thr'])
                    else:
                        S.op('dve', MS(THR[:, 0:2], -1e29), writes=['thr'])
                    for t_ in range(2):
                        ncols = (i0 + t_ + 1) * 128
                        S.op('dve', TS(M01[t_][:, 0:ncols], IDX2[t_][:, 0:ncols], THR[:, t_:t_ + 1], None, ALU.is_ge),
                             reads=['idx%d' % t_, 'thr'], writes=['m01_%d' % t_])

                def stage1_tr(c, q0):
                    MT = MT2[c % 2]
                    mk_ = 'mt%d' % (c % 2)
                    nj = 4 * c + 4
                    for t_ in range(2):
                        q = q0 + t_
                        i = c * 4 + q
                        for j0 in range(0, i + 1, 8):
                            jn = min(8, i + 1 - j0)
                            S.group('pe', [TR(PTB[:, jj * 128:(jj + 1) * 128], M01[t_][:, (j0 + jj) * 128:(j0 + jj + 1) * 128], ident_b[:])
                                           for jj in range(jn)], reads=['m01_%d' % t_, 'ident_b'], writes=['ptb'])
                            S.op('act', ACTV(MT[:, j0:j0 + jn, q * 128:(q + 1) * 128],
                                             PTB[:, 0:jn * 128].rearrange("p (j t) -> p j t", j=jn), AF.Copy),
                                 reads=['ptb'], writes=[mk_])
                        if i + 1 < nj:
                            S.op('pool', MS(MT[:, i + 1:nj, q * 128:(q + 1) * 128], 0.0), writes=[mk_])

                def stage2_proj(c):
                    ts_ = slice(c * 512, (c + 1) * 512)
                    hk = HTK(c)
                    for g in range(4):
                        pq = g % 2
                        S.group('pe', [MM(PB[pq][:, :], WBt[:, k, g * 128:(g + 1) * 128], HT[:, k, ts_], k == 0, k == 7) for k in range(8)],
                                reads=hk + ['wb'], writes=[pbk[pq]])
                        S.op('act', ACTV(QD[:, g, :], PB[pq][:, :], AF.Copy), reads=[pbk[pq]], writes=['qd'])

                bic = [0]

                def stage2_heads(c, heads):
                    ts_ = slice(c * 512, (c + 1) * 512)
                    MT = MT2[c % 2]
                    mk_ = 'mt%d' % (c % 2)
                    nj = 4 * c + 4
                    blocks = [(hd, j) for hd in heads for j in range(nj)]
                    LA = 3
                    base = bic[0]
                    bic[0] += len(blocks)

                    def emit_score(bi):
                        hd, j = blocks[bi]
                        kv, g = hd // 4, hd % 4
                        ps = 2 + ((base + bi) % 3)
                        d = (base + bi) % 3
                        S.op('act', ACTV(DT[d][:], POSF[:, ts_], AF.Abs, bias=NPC[:, j:j + 1]), reads=['posf', 'npc'], writes=['dt%d' % d])
                        S.group('pe', [MM(PB[ps][:, :], KD[kv * 64:(kv + 1) * 64, j * 128:(j + 1) * 128], QD[kv * 64:(kv + 1) * 64, g, :], True, False),
                                       MM(PB[ps][:, :], SLD[:, hd, :], DT[d][:], False, True)],
                                reads=['kd%d' % (j // 4), 'qd', 'sld', 'dt%d' % d], writes=[pbk[ps]])
                    for bi in range(min(LA, len(blocks))):
                        emit_score(bi)
                    for bi, (hd, j) in enumerate(blocks):
                        kv = hd // 4
                        po = 5 + (hd % 2)
                        ps = 2 + ((base + bi) % 3)
                        d = (base + bi) % 3
                        S.op('act', ACTV(EE[d][:], PB[ps][:, :], AF.Exp, scale=0.125), reads=[pbk[ps]], writes=['ee%d' % d])
                        S.op('pool', TT(PM[d][:], EE[d][:], MT[:, j, :], ALU.mult), reads=['ee%d' % d, mk_], writes=['pm%d' % d])
                        S.group('pe', [MM(PB[po][:, :], VD[:, j, kv, :], PM[d][:], j == 0, j == nj - 1)],
                                reads=['vd', 'pm%d' % d], writes=[pbk[po]])
                        if bi + LA < len(blocks):
                            emit_score(bi + LA)
                        if j == nj - 1:
                            rt = RT[hd % 2]
                            rk = 'drt%d' % (hd % 2)
                            S.op('dve', TS(rt[0:64, :], PB[po][64:128, :], 1e-30, None, ALU.max), reads=[pbk[po]], writes=[rk])
                            S.op('dve', lambda e, rt=rt: e.reciprocal(out=rt[0:64, :], in_=rt[0:64, :]), reads=[rk], writes=[rk])
                            S.op('dve', TT(MIX[(hd % 2) * 64:(hd % 2) * 64 + 64, 4 + hd // 2, ts_], PB[po][0:64, :], rt[0:64, :], ALU.mult),
                                 reads=[pbk[po], rk], writes=['mix%d' % c])

                stage1_proj(0)
                for q0 in (0, 2):
                    stage1_idx(0, q0)
                    stage1_bisect(0, q0)
                    stage1_tr(0, q0)
                for c in range(NC4):
                    stage2_proj(c)
                    nxt = c + 1 < NC4
                    if nxt:
                        stage1_proj(c + 1)
                        stage1_idx(c + 1, 0)
                        stage1_bisect(c + 1, 0)
                    stage2_heads(c, [0, 1, 2, 3])
                    if nxt:
                        stage1_tr(c + 1, 0)
                        stage1_idx(c + 1, 2)
                        stage1_bisect(c + 1, 2)
                    stage2_heads(c, [4, 5, 6, 7])
                    if nxt:
                        stage1_tr(c + 1, 2)
                dump("mix_all", MIX[:], [128, 8, S_LEN], BF16, reads=['mix%d' % c for c in range(4)])
                S.barrier()
        if stage <= 4:
            S.finish(); S.replay()
            return nc, dbg_out

        regc = {}

        def breg(e):
            if 'r' not in regc:
                regc['r'] = e.to_reg(NEXP * CAP - 1)
            return regc['r']
        BIG = float(NSL + 1)
        ph2 = ExitStack()
        AB2 = T(ph2, "ab2", [128, D], F32)
        BB2 = T(ph2, "bb2", [128, D], F32)
        H2TM = T(ph2, "h2tm", [128, NT, D], BF16)
        with ExitStack() as p5:
            WO = T(p5, "wo", [128, 8, D], BF16)
            cwo = S.chan('wo')
            S.dma('pool', cwo, DMA(WO[:], w_o_d.rearrange("(k p) n -> p k n", p=128)), writes=['wo'])
            BT = [T(p5, "bt%d" % i, [128, 128], F32) for i in range(2)]
            for vi, (colv, dstv) in enumerate([(A2, AB2), (B2c, BB2)]):
                for k in range(8):
                    bi = (vi * 8 + k) % 2
                    S.op('dve', TS(BT[bi][:], ones_f[:], colv[:, k:k + 1], None, ALU.mult), reads=['ones_f', 'cols'], writes=['bt%d' % bi])
                    S.group('pe', [MM(PB[4 + bi][:, 0:128], BT[bi][:], ident_f[:])], reads=['bt%d' % bi, 'ident_f'], writes=[pbk[4 + bi]])
                    S.op('act', ACTV(dstv[:, k * 128:(k + 1) * 128], PB[4 + bi][:, 0:128], AF.Copy), reads=[pbk[4 + bi]], writes=['abb%d' % vi])
            XR = [T(p5, "xr%d" % i, [128, D], F32) for i in range(2)]
            TM = [T(p5, "tm%d" % i, [128, D], F32) for i in range(2)]
            JK = T(p5, "jk", [128, 512], BF16)
            cxr = [S.chan('xr0'), S.chan('xr1')]
            cx1 = [S.chan('x1st%d' % i) for i in range(4)]
            st5 = T(p5, "st5", [128, 8], F32)

            def load_x1(tt, buf, key):
                b = tt % 2
                S.dma('sp', cxr[b], DMA(XR[b][:], x_d[tt * 128:(tt + 1) * 128, :]), writes=['xr%d' % b])
                for hf in range(2):
                    pbi = 2 * b + hf
                    S.group('pe', [MM(PB[pbi][:, :], MIX[:, k, tt * 128:(tt + 1) * 128], WO[:, k, hf * 512:(hf + 1) * 512], k == 0, k == 7)
                                   for k in range(8)], reads=['wo', 'mix%d' % (tt // 4)], writes=[pbk[pbi]])
                    S.op('act', ACTV(JK[:], PB[pbi][:, :], AF.Square, accum_out=st5[:, hf:hf + 1]),
                         reads=[pbk[pbi]], writes=['jk', 'st5_%d' % hf])
                S.op('dve', TT(st5[:, 2:3], st5[:, 0:1], st5[:, 1:2], ALU.add), reads=['st5_0', 'st5_1'], writes=['st5s'])
                S.op('act', ACTV(st5[:, 3:4], st5[:, 2:3], AF.Sqrt, scale=1.0 / D, bias=epsc[:, 0:1]), reads=['st5s', 'epsc'], writes=['st5q'])
                S.op('dve', lambda e: e.reciprocal(out=st5[:, 4:5], in_=st5[:, 3:4]), reads=['st5q'], writes=['st5r'])
                for hf in range(2):
                    pbi = 2 * b + hf
                    S.op('dve', STT(TM[b][:, hf * 512:(hf + 1) * 512], PB[pbi][:, :], st5[:, 4:5], G1[:, hf * 512:(hf + 1) * 512],
                                    ALU.mult, ALU.mult), reads=[pbk[pbi], 'st5r', 'g0'], writes=['tm%d' % b])
                S.op('dve', TT(buf[:], TM[b][:], XR[b][:], ALU.add), reads=['tm%d' % b, 'xr%d' % b], writes=[key])
                S.dma('sp', cx1[tt % 4], DMA(x1_d[tt * 128:(tt + 1) * 128, :], buf[:]), reads=[key])

            def mk_h2tm(tt, xn, key):
                b = tt % 2
                S.op('dve', TT(TM[b][:], xn[:], AB2[:], ALU.mult), reads=[key, 'abb0', 'tm%d' % b], writes=['tm%d' % b])
                S.op('dve', TT(H2TM[:, tt, :], TM[b][:], BB2[:], ALU.add), reads=['tm%d' % b, 'abb1'], writes=['h2tm'])
            p6s = p5
            WR = T(p6s, "wr", [128, 8, NEXP], BF16)
            BR = T(p6s, "br", [128, NEXP], F32)
            LG = T(p6s, "lg", [128, NEXP], F32)
            EX = T(p6s, "ex", [128, NEXP], F32)
            MK = T(p6s, "mk", [128, NEXP], F32)
            MKB = T(p6s, "mkb", [128, NT, NEXP], BF16)
            ones_b = T(p6s, "ones_b", [128, 128], BF16)
            tris_b = T(p6s, "tris_b", [128, 128], BF16)
            CUM = T(p6s, "cum", [128, NT, NEXP], F32)
            VAL = T(p6s, "val", [128, NT, NEXP], F32)
            KEY = T(p6s, "key", [128, NT, NEXP], F32)
            EOI = T(p6s, "eoi", [128, NT, NEXP], I32)
            EOF_ = T(p6s, "eof", [128, NT, NEXP], F32)
            K8 = T(p6s, "k8", [128, NT, 8], F32)
            SLF = T(p6s, "slf", [128, NT, 4], F32)
            JK6 = T(p6s, "jk6", [128, NEXP], F32)
            S.op('pool', MS(ones_b[:], 1.0), writes=['ones_b'])
            S.op('pool', MS(tris_b[:], 1.0), writes=['tris_b'])
            S.op('pool', lambda e: e.affine_select(out=tris_b[:], in_=tris_b[:], pattern=[[1, 128]],
                                                   compare_op=ALU.is_gt, fill=0.0, base=0, channel_multiplier=-1),
                 reads=['tris_b'], writes=['tris_b'])
            S.op('pool', lambda e: e.iota(EOI[:], pattern=[[0, NT], [CAP, NEXP]], base=0, channel_multiplier=0),
                 writes=['eoi'])
            S.op('dve', CP(EOF_[:], EOI[:]), reads=['eoi'], writes=['eof'])
            S.dma('pool', S.chan(), DMA(WR[:], w_r_d.rearrange("(k p) n -> p k n", p=128)), writes=['wr'])
            S.dma('sp', S.chan(), DMA(BR[:], b_r_d.partition_broadcast(128)), writes=['br'])
            cscl = [S.chan('scat%d' % i) for i in range(8)]
            S.hold = set()
            S._emit_waits('pool', {cz: S.count[cz]})

            def dispatch_chunk(tc):
                c4 = slice(tc * 4, (tc + 1) * 4)
                for tt in range(tc * 4, tc * 4 + 4):
                    S.group('pe', [MM(PB[5][:, 0:32], HT[:, k, tt * 128:(tt + 1) * 128], WR[:, k, :], k == 0, k == 7) for k in range(8)],
                            reads=['wr', 'g_d%d' % tc], writes=[pbk[5]])
                    S.op('dve', TT(LG[:], PB[5][:, 0:32], BR[:], ALU.add), reads=[pbk[5], 'br'], writes=['lg'])
                    S.op('dve', lambda e: e.max(out=rt6[:, 0:8], in_=LG[:]), reads=['lg'], writes=['r6a'])
                    S.op('dve', TS(rt6[:, 8:9], rt6[:, 0:1], -1.0, None, ALU.mult), reads=['r6a'], writes=['r6b'])
                    S.op('act', ACTV(EX[:], LG[:], AF.Exp, bias=rt6[:, 8:9]), reads=['lg', 'r6b'], writes=['ex'])
                    S.op('dve', TS(MK[:], LG[:], rt6[:, 3:4], None, ALU.is_ge), reads=['lg', 'r6a'], writes=['mk'])
                    S.op('dve', CP(MKB[:, tt, :], MK[:]), reads=['mk'], writes=['mkb'])
                    S.op('dve', STT(EX[:], EX[:], 1.0, MK[:], ALU.mult, ALU.mult, accum_out=rt6[:, 9:10]),
                         reads=['ex', 'mk'], writes=['ex', 'r6c'])
                    S.op('dve', lambda e: e.reciprocal(out=rt6[:, 10:11], in_=rt6[:, 9:10]), reads=['r6c'], writes=['r6d'])
                    S.op('dve', TS(GATE[:, tt, :], EX[:], rt6[:, 10:11], None, ALU.mult), reads=['ex', 'r6d'], writes=['gate'])
                for tt in range(tc * 4, tc * 4 + 4):
                    fns = [MM(PB[6][:, tt * 32:(tt + 1) * 32], ones_b[:], MKB[:, t2, :], t2 == 0, False) for t2 in range(tt)]
                    fns.append(MM(PB[6][:, tt * 32:(tt + 1) * 32], tris_b[:], MKB[:, tt, :], tt == 0, True))
                    S.group('pe', fns, reads=['mkb', 'ones_b', 'tris_b'], writes=[pbk[6]])
                pv = PB[6][:, tc * 128:(tc + 1) * 128].rearrange("p (a e) -> p a e", a=4)
                S.op('dve', CP(CUM[:, c4, :], pv), reads=[pbk[6]], writes=['cum'])
                S.op('dve', TS(VAL[:, c4, :], CUM[:, c4, :], float(CAP), None, ALU.is_lt), reads=['cum'], writes=['val'])
                S.op('dve', TT(GATE[:, c4, :], GATE[:, c4, :], VAL[:, c4, :], ALU.mult), reads=['gate', 'val'], writes=['gate'])
                S.op('dve', TT(KEY[:, c4, :], CUM[:, c4, :], EOF_[:, c4, :], ALU.add), reads=['cum', 'eof'], writes=['key'])
                S.op('dve', TS(KEY[:, c4, :], KEY[:, c4, :], -1.0, BIG, ALU.mult, ALU.add), reads=['key'], writes=['key'])
                S.op('dve', TS(VAL[:, c4, :], GATE[:, c4, :], 0.0, None, ALU.is_gt), reads=['gate'], writes=['val'])
                S.op('dve', TT(KEY[:, c4, :], KEY[:, c4, :], VAL[:, c4, :], ALU.mult), reads=['key', 'val'], writes=['key'])
                for tt in range(tc * 4, tc * 4 + 4):
                    S.op('dve', lambda e, tt=tt: e.max(out=K8[:, tt, :], in_=KEY[:, tt, :]), reads=['key'], writes=['k8'])
                S.op('dve', TS(SLF[:, c4, :], K8[:, c4, 0:4], -1.0, BIG, ALU.mult, ALU.add), reads=['k8'], writes=['slf'])
                S.op('dve', CP(IDXS[:, c4, :], SLF[:, c4, :]), reads=['slf'], writes=['idxs'])
                for tt in range(tc * 4, tc * 4 + 4):
                    for k in range(4):
                        S.op('dve', STT(JK6[:], KEY[:, tt, :], K8[:, tt, k:k + 1], GATE[:, tt, :], ALU.is_equal, ALU.mult,
                                        accum_out=GK[:, tt, k:k + 1]), reads=['key', 'k8', 'gate', 'jk6'], writes=['jk6', 'gk%d_%d' % (tt, k)])
                for tt in range(tc * 4, tc * 4 + 4):
                    for k in range(4):
                        S.dma('pool', cscl[(tt * 4 + k) % 8], lambda e, tt=tt, k=k: e.indirect_dma_start(
                            out=xs_d[:, :], out_offset=bass.IndirectOffsetOnAxis(ap=IDXS[:, tt, k:k + 1], axis=0),
                            in_=H2TM[:, tt, :], in_offset=None, bounds_check=breg(e), oob_is_err=False),
                            reads=['xs', 'idxs', 'h2tm'], writes=['xs_sc%d_%d' % (tt, k)], drain=True)
            norm_transpose(p5, load_x1, A2, B2c, 'g', HT, pre=mk_h2tm, post=dispatch_chunk)
            dump("h2t", HT[:], [128, 8, S_LEN], BF16, reads=['g_d%d' % i for i in range(4)])
            S.barrier()
        if stage <= 5:
            S.finish(); S.replay()
            return nc, dbg_out

        ph2.close()
        pmx.close()

        n_exp = NEXP if stage >= 7 else 2
        NSLOT = 6
        w1v = w1_d.rearrange("e (k p) n -> e p k n", p=128)
        w2v = w2_d.rearrange("e (k p) n -> e p k n", p=128)
        pieces = []
        for e_ in range(n_exp):
            for piece in range(4):
                pieces.append((e_, 0, piece))
            for dh in range(2):
                pieces.append((e_, 1, dh))
        with ExitStack() as p6e:
            WS = [T(p6e, "ws%d" % i, [128, 8, 512], BF16) for i in range(NSLOT)]
            cws = [S.chan('ws%d' % i) for i in range(NSLOT)]

            def issue(pi):
                if pi >= len(pieces):
                    return
                e_, kind, idx = pieces[pi]
                sl = pi % NSLOT
                src = (w1v if kind == 0 else w2v)[e_, :, :, idx * 512:(idx + 1) * 512]
                S.dma('pool', cws[sl], DMA(WS[sl][:], src), writes=['ws%d' % sl])
            for pi in range(NSLOT - 1):
                issue(pi)
            B1R = T(p6e, "b1r", [32, 2 * D], F32)
            B1C = T(p6e, "b1c", [128, 16, NEXP], F32)
            B1P = T(p6e, "b1p", [128, 16, NEXP], F32)
            S.dma('sp', S.chan(), DMA(B1R[:], b1_d), writes=['b1r'])
            b1v = B1R[:].rearrange("e (fc p two) -> e fc two p", fc=8, two=2)
            fns = []
            for fc in range(8):
                for two in range(2):
                    s_ = fc * 2 + two
                    fns.append(TR(PB[6][:, s_ * 32:(s_ + 1) * 32], b1v[:, fc, two, :], ident_f[0:32, 0:32]))
            S.group('pe', fns, reads=['b1r', 'ident_f'], writes=[pbk[6]])
            S.op('dve', CP(B1C[:].rearrange("p s e -> p (s e)"), PB[6][:, :]), reads=[pbk[6]], writes=['b1c'])
            S.op('dve', TS(B1P[:].rearrange("p s e -> p (s e)"), PB[6][:, :], 1.0, None, ALU.add), reads=[pbk[6]], writes=['b1c'])
            XE = [T(p6e, "xe%d" % i, [128, 4, D], BF16) for i in range(2)]
            XT = [T(p6e, "xt%d" % i, [128, 8, CAP], BF16) for i in range(2)]
            cxe = [S.chan('xe0'), S.chan('xe1')]
            GG = [T(p6e, "gg%d" % i, [128, 512], F32) for i in range(2)]
            SG = [T(p6e, "sg%d" % i, [128, 512], F32) for i in range(2)]
            LL = [T(p6e, "ll%d" % i, [128, 512], F32) for i in range(2)]
            ACT2 = [T(p6e, "act2_%d" % i, [128, 8, CAP], BF16) for i in range(2)]
            YB = [T(p6e, "yb%d" % i, [128, 4, D], BF16) for i in range(2)]
            cyb = [S.chan('yb0'), S.chan('yb1')]

            def load_xe(e_):
                if e_ >= n_exp:
                    return
                b = e_ % 2
                S.dma('sp', cxe[b], DMA(XE[b][:], xs_d[e_ * CAP:(e_ + 1) * CAP, :].rearrange("(a p) n -> p a n", p=128)),
                      writes=['xe%d' % b])

            def transposes(e_):
                if e_ >= n_exp:
                    return
                b = e_ % 2
                for st in range(4):
                    tb_, tk_ = (PTB[:, :], 'ptb') if st % 2 == 0 else (PB[6][:, :].bitcast(BF16), pbk[6])
                    S.group('pe', [TR(tb_[:, k * 128:(k + 1) * 128], XE[b][:, st, k * 128:(k + 1) * 128], ident_b[:]) for k in range(8)],
                            reads=['xe%d' % b, 'ident_b'], writes=[tk_])
                    if st % 2 == 0:
                        S.op('act', ACTV(XT[b][:, :, st * 128:(st + 1) * 128], tb_.rearrange("p (k s) -> p k s", k=8), AF.Copy),
                             reads=[tk_], writes=['xt%d' % b])
                    else:
                        S.op('act', ACTV(XT[b][:, :, st * 128:(st + 1) * 128], tb_.rearrange("p (k s) -> p k s", k=8), AF.Copy),
                             reads=[tk_], writes=['xt%d' % b])
            load_xe(0)
            load_xe(1)
            transposes(0)
            it = 0
            yi = 0
            pi = 0
            for e_ in range(n_exp):
                b = e_ % 2
                for piece in range(4):
                    sl = pi % NSLOT
                    issue(pi + NSLOT - 1)
                    pi += 1
                    for fcl in range(2):
                        fc = piece * 2 + fcl
                        bb = it % 2
                        it += 1
                        pg, pl = 2 * bb, 2 * bb + 1
                        S.group('pe', [MM(PB[pg][:, :], WS[sl][:, k, fcl * 256:fcl * 256 + 256:2], XT[b][:, k, :], k == 0, k == 7) for k in range(8)],
                                reads=['ws%d' % sl, 'xt%d' % b], writes=[pbk[pg]])
                        S.group('pe', [MM(PB[pl][:, :], WS[sl][:, k, fcl * 256 + 1:fcl * 256 + 256:2], XT[b][:, k, :], k == 0, k == 7) for k in range(8)],
                                reads=['ws%d' % sl, 'xt%d' % b], writes=[pbk[pl]])
                        S.op('dve', TS(GG[bb][:], PB[pg][:, :], B1C[:, fc * 2, e_:e_ + 1], 7.0, ALU.add, ALU.min),
                             reads=[pbk[pg], 'b1c'], writes=['gg%d' % bb])
                        S.op('act', ACTV(SG[bb][:], GG[bb][:], AF.Silu, scale=1.702), reads=['gg%d' % bb], writes=['sg%d' % bb])
                        S.op('act', ACTV(LL[bb][:], PB[pl][:, :], AF.Identity, bias=B1P[:, fc * 2 + 1, e_:e_ + 1]),
                             reads=[pbk[pl], 'b1c'], writes=['ll%d' % bb])
                        S.op('dve', TS(LL[bb][:], LL[bb][:], 8.0, -6.0, ALU.min, ALU.max), reads=['ll%d' % bb], writes=['ll%d' % bb])
                        S.op('dve', STT(ACT2[b][:, fc, :], SG[bb][:], 1.0 / 1.702, LL[bb][:], ALU.mult, ALU.mult),
                             reads=['sg%d' % bb, 'll%d' % bb], writes=['act2_%d' % b])
                transposes(e_ + 1)
                for dh in range(2):
                    sl = pi % NSLOT
                    issue(pi + NSLOT - 1)
                    pi += 1
                    for st in range(4):
                        py = 4 + (yi % 2)
                        yi += 1
                        S.group('pe', [MM(PB[py][:, :], ACT2[b][:, f, st * 128:(st + 1) * 128], WS[sl][:, f, :], f == 0, f == 7) for f in range(8)],
                                reads=['ws%d' % sl, 'act2_%d' % b], writes=[pbk[py]])
                        if yi % 2:
                            S.op('act', ACTV(YB[b][:, st, dh * 512:(dh + 1) * 512], PB[py][:, :], AF.Copy), reads=[pbk[py]], writes=['yb%d' % b])
                        else:
                            S.op('dve', CP(YB[b][:, st, dh * 512:(dh + 1) * 512], PB[py][:, :]), reads=[pbk[py]], writes=['yb%d' % b])
                S.dma('sp', cyb[b], DMA(ys_d[e_ * CAP:(e_ + 1) * CAP, :].rearrange("(a p) n -> p a n", p=128), YB[b][:]),
                      reads=['yb%d' % b], writes=['ys%d' % e_])
                load_xe(e_ + 2)
            S.barrier()

        with ExitStack() as p6f:
            GATET = T(p6f, "gatet", [32, S_LEN], F32)
            B2R = T(p6f, "b2r", [32, D], F32)
            S.dma('sp', S.chan(), DMA(B2R[:], b2_d), writes=['b2r'])
            for g4 in range(4):
                S.group('pe', [TR(PB[4][0:32, q * 128:(q + 1) * 128], GATE[:, g4 * 4 + q, :], ident_f[:]) for q in range(4)],
                        reads=['gate', 'ident_f'], writes=[pbk[4]])
                S.op('act', ACTV(GATET[0:32, g4 * 512:(g4 + 1) * 512], PB[4][0:32, :], AF.Copy), reads=[pbk[4]], writes=['gatet'])
            YG = [[T(p6f, "yg%d_%d" % (k, i), [128, D], BF16) for i in range(2)] for k in range(4)]
            cyg = [[S.chan('yg%d_%d' % (k, i)) for i in range(2)] for k in range(4)]
            for k in range(4):
                for i in range(2):
                    S.op('pool', MS(YG[k][i][:], 0.0), writes=['yg%d_%d' % (k, i)])
            AC = [T(p6f, "ac%d" % i, [128, D], F32) for i in range(2)]
            X1 = [T(p6f, "x1b%d" % i, [128, D], F32) for i in range(2)]
            OT = [T(p6f, "ot%d" % i, [128, D], F32) for i in range(2)]
            JK2 = T(p6f, "jk2", [128, D], BF16)
            cl = [S.chan('x1l0'), S.chan('x1l1')]
            co = [S.chan('o0'), S.chan('o1')]
            for tt in range(NT):
                b = tt % 2
                S.dma('sp', cl[b], DMA(X1[b][:], x1_d[tt * 128:(tt + 1) * 128, :]), writes=['x1b%d' % b])
                for k in range(4):
                    S.dma('pool', cyg[k][b], lambda e, tt=tt, k=k, b=b: e.indirect_dma_start(
                        out=YG[k][b][:, :], out_offset=None, in_=ys_d[:, :],
                        in_offset=bass.IndirectOffsetOnAxis(ap=IDXS[:, tt, k:k + 1], axis=0),
                        bounds_check=breg(e), oob_is_err=False), writes=['yg%d_%d' % (k, b)], drain=True)
                for hf in range(2):
                    pbi = 2 * b + hf
                    S.group('pe', [MM(PB[pbi][:, :], GATET[0:32, tt * 128:(tt + 1) * 128], B2R[0:32, hf * 512:(hf + 1) * 512])],
                            reads=['gatet', 'b2r'], writes=[pbk[pbi]])
                    S.op('dve', STT(AC[b][:, hf * 512:(hf + 1) * 512], YG[0][b][:, hf * 512:(hf + 1) * 512], GK[:, tt, 0:1], PB[pbi][:, :],
                                    ALU.mult, ALU.add), reads=['yg0_%d' % b, pbk[pbi]], writes=['ac%d' % b])
                for k in range(1, 4):
                    S.op('dve', STT(AC[b][:], YG[k][b][:], GK[:, tt, k:k + 1], AC[b][:], ALU.mult, ALU.add),
                         reads=['yg%d_%d' % (k, b), 'ac%d' % b], writes=['ac%d' % b])
                if dbg and tt == 0:
                    dump("acc0", AC[0][:], [128, D], reads=['ac0'])
                S.op('act', ACTV(JK2[:], AC[b][:], AF.Square, accum_out=rt6[:, 16:17]), reads=['ac%d' % b], writes=['jk2', 'f6a'])
                S.op('act', ACTV(rt6[:, 17:18], rt6[:, 16:17], AF.Sqrt, scale=1.0 / D, bias=epsc[:, 0:1]), reads=['f6a', 'epsc'], writes=['f6b'])
                S.op('dve', lambda e: e.reciprocal(out=rt6[:, 18:19], in_=rt6[:, 17:18]), reads=['f6b'], writes=['f6c'])
                S.op('dve', STT(OT[b][:], AC[b][:], rt6[:, 18:19], G2[:], ALU.mult, ALU.mult), reads=['ac%d' % b, 'f6c', 'g1'], writes=['ot%d' % b])
                S.op('dve', TT(OT[b][:], OT[b][:], X1[b][:], ALU.add), reads=['ot%d' % b, 'x1b%d' % b], writes=['ot%d' % b])
                S.dma('sp', co[b], DMA(out_d[tt * 128:(tt + 1) * 128, :], OT[b][:]), reads=['ot%d' % b])
        S.finish()
        S.replay()
    return nc, dbg_out


_FRQ = None


def _host_inputs(inputs, b):
    f = lambda k: np.ascontiguousarray(np.asarray(inputs[k]))
    pos = np.asarray(inputs["positions"])[b].astype(np.int32)
    frq = np.zeros((128, 1), np.float32)
    j = np.arange(16, dtype=np.float64)
    fr = (10000.0 ** (-j / 16.0)) / (2 * np.pi)
    frq[64:80, 0] = fr
    frq[80:96, 0] = fr
    g4 = np.concatenate([f("g_pre_mix")[0], f("g_post_mix")[0], f("g_pre_ffn")[0], f("g_post_ffn")[0]])[None, :]
    return {
        "x": f("x")[b],
        "c": np.ascontiguousarray(f("c")[b].reshape(8, 128).T),
        "posr": np.ascontiguousarray(pos),
        "posc": np.ascontiguousarray(pos.reshape(NT, 128).T),
        "frq": frq,
        "w_ada": f("w_ada")[0], "b_ada": f("b_ada"),
        "g4": np.ascontiguousarray(g4),
        "w_in": f("w_in")[0],
        "gq": np.ascontiguousarray(f("g_q_a")[0].reshape(2, 128).T),
        "gkv": np.ascontiguousarray(f("g_kv_a")[0].reshape(1, 128).T),
        "w_q_b": f("w_q_b")[0], "w_kv_b": f("w_kv_b")[0], "w_o": f("w_o")[0],
        "w_router": f("w_router")[0], "b_router": f("b_router")[0],
        "w_mlp1": f("w_mlp1")[0], "b_mlp1": f("b_mlp1")[0],
        "w_mlp2": f("w_mlp2")[0], "b_mlp2": f("b_mlp2")[0],
    }


def kernel(**inputs):
    nc, _ = build_program(stage=99, dbg=False)
    in_maps = [_host_inputs(inputs, b) for b in range(8)]
    res = run_bass_kernel_spmd(nc, in_maps, core_ids=list(range(8)))
    out = np.stack([np.asarray(r["out"]) for r in res.results], axis=0)
    return out.astype(np.float32)
```
